# Optimizing a Trainium2 kernel written in Bass

```python
import math
import jax, jax.numpy as jnp
from jax import lax
import numpy as np

D_MODEL = 1024
BATCH = 4
SEQ = 8192
DEPTH = 2

HEAD_DIM = 64
N_ATTN_HEADS = D_MODEL // HEAD_DIM
N_DIFF_HEADS = N_ATTN_HEADS // 4
DIL_GROUPS = ((128, 1), (512, 4), (2048, 16))
HEADS_PER_DIL = (N_ATTN_HEADS - N_DIFF_HEADS) // len(DIL_GROUPS)
N_DIL_HEADS = HEADS_PER_DIL * len(DIL_GROUPS)
DIL_WIDTH = N_DIL_HEADS * HEAD_DIM
DIFF_QK_DIM = HEAD_DIM // 2
DIFF_QK_WIDTH = N_DIFF_HEADS * 2 * DIFF_QK_DIM
DIFF_V_WIDTH = N_DIFF_HEADS * HEAD_DIM
ATTN_IN_WIDTH = 3 * DIL_WIDTH + 2 * DIFF_QK_WIDTH + DIFF_V_WIDTH
ATTN_SPLITS = [DIL_WIDTH, 2 * DIL_WIDTH, 3 * DIL_WIDTH,
               3 * DIL_WIDTH + DIFF_QK_WIDTH, 3 * DIL_WIDTH + 2 * DIFF_QK_WIDTH]
BLOCK = 128
HGRN_HEADS = 8
HGRN_EXPAND = 128
HGRN_FORGET_DIM = HGRN_HEADS * HGRN_EXPAND
HGRN_VDIM = D_MODEL // HGRN_HEADS
HGRN_CHUNK = 64
HGRN_IN_WIDTH = 2 * HGRN_FORGET_DIM + 2 * D_MODEL
D_FF = 2816
N_EXPERTS = 8
TOP_K = 2
D_FF_EXPERT = 3584
N_EVEN = (DEPTH + 1) // 2
N_ODD = DEPTH // 2
EPS = 1e-6
NEG_INF = -1e30
F32 = jnp.float32

kernel_name = "hybrid_dilated_diffattn_hgrn2_moe"


def rms_norm(x, gain):
    xf = x.astype(F32)
    y = xf * lax.rsqrt(jnp.mean(xf * xf, axis=-1, keepdims=True) + EPS)
    return (y * gain.astype(F32)).astype(x.dtype)


def alibi_slopes(n):
    return 2.0 ** (-8.0 * (jnp.arange(n, dtype=F32) + 1.0) / n)


def dilated_window_attention(q, k, v, slopes, window, dilation):
    B, S, H, Dh = q.shape
    span = window // dilation
    assert span <= BLOCK
    n_blk = -(-S // (dilation * BLOCK))
    Lp = n_blk * dilation * BLOCK

    def to_classes(t):
        t = jnp.pad(t, ((0, 0), (0, Lp - S), (0, 0), (0, 0)))
        t = t.reshape(B, Lp // dilation, dilation, H, Dh).transpose(0, 2, 1, 3, 4)
        return t.reshape(B, dilation, n_blk, BLOCK, H, Dh)

    def with_prev(t):
        prev = jnp.pad(t[:, :, :-1], ((0, 0), (0, 0), (1, 0), (0, 0), (0, 0), (0, 0)))
        return jnp.concatenate([prev, t], axis=3)

    qc = to_classes(q)
    kb = with_prev(to_classes(k))
    vb = with_prev(to_classes(v))
    step = jnp.arange(BLOCK)[:, None] + BLOCK - jnp.arange(2 * BLOCK)[None, :]
    not_before_start = (jnp.arange(n_blk)[:, None, None] > 0) | (jnp.arange(2 * BLOCK)[None, None, :] >= BLOCK)
    valid = (step >= 0) & (step <= span) & not_before_start
    bias = -slopes[:, None, None] * (step * dilation).astype(F32)
    s = jnp.einsum('brnqhd,brnkhd->brnhqk', qc, kb).astype(F32) * (Dh ** -0.5) + bias
    s = jnp.where(valid[None, None, :, None], s, NEG_INF)
    lse = jax.nn.logsumexp(s, axis=-1)
    p = jnp.exp(s - lse[..., None])
    o = jnp.einsum('brnhqk,brnkhd->brnqhd', p, vb.astype(F32))

    def from_classes(t):
        X = t.shape[-1]
        t = t.reshape(B, dilation, Lp // dilation, H, X).transpose(0, 2, 1, 3, 4)
        return t.reshape(B, Lp, H, X)[:, :S]

    lse_pos = from_classes(jnp.swapaxes(lse, -1, -2)[..., None])[..., 0]
    return from_classes(o), lse_pos


def differential_attention(q1, q2, k1, k2, v, slopes, lam):
    B, S, H, dk = q1.shape
    n_blk = S // BLOCK
    scale = dk ** -0.5
    kpos = jnp.arange(S)
    vf = v.astype(F32)

    def one_block(args):
        qb1, qb2, blk = args
        dist = (blk * BLOCK + jnp.arange(BLOCK))[:, None] - kpos[None, :]
        causal = dist >= 0
        bias = -slopes[:, None, None] * dist.astype(F32)

        def probs(qb, kk):
            s = jnp.einsum('bqhd,bkhd->bhqk', qb, kk).astype(F32) * scale + bias
            return jax.nn.softmax(jnp.where(causal, s, NEG_INF), axis=-1)

        a = probs(qb1, k1) - lam * probs(qb2, k2)
        return jnp.einsum('bhqk,bkhd->bqhd', a, vf)

    to_blocks = lambda t: t.reshape(B, n_blk, BLOCK, H, dk).transpose(1, 0, 2, 3, 4)
    out = lax.map(one_block, (to_blocks(q1), to_blocks(q2), jnp.arange(n_blk)))
    return out.transpose(1, 0, 2, 3, 4).reshape(B, S, H, 2 * dk)


def dilated_diff_mixer(u, w_in, dil_q_gain, dil_k_gain, diff_q_gain, diff_k_gain,
                       lam_q1, lam_k1, lam_q2, lam_k2, diff_out_gain, w_out, layer):
    B, S, _ = u.shape
    dq, dk, dv, fq, fk, fv = jnp.split(u @ w_in, ATTN_SPLITS, axis=-1)
    slopes = alibi_slopes(N_ATTN_HEADS)
    dq = rms_norm(dq.reshape(B, S, N_DIL_HEADS, HEAD_DIM), dil_q_gain)
    dk = rms_norm(dk.reshape(B, S, N_DIL_HEADS, HEAD_DIM), dil_k_gain)
    dv = dv.reshape(B, S, N_DIL_HEADS, HEAD_DIM)
    outs, lses = [], []
    for g, (window, dilation) in enumerate(DIL_GROUPS):
        hs = slice(g * HEADS_PER_DIL, (g + 1) * HEADS_PER_DIL)
        o, lse = dilated_window_attention(dq[:, :, hs], dk[:, :, hs], dv[:, :, hs], slopes[hs], window, dilation)
        outs.append(o)
        lses.append(lse)
    alpha = jax.nn.softmax(jnp.stack(lses), axis=0)
    dil_out = jnp.concatenate([alpha[g][..., None] * outs[g] for g in range(len(DIL_GROUPS))], axis=2)
    fq = rms_norm(fq.reshape(B, S, N_DIFF_HEADS, 2, DIFF_QK_DIM), diff_q_gain)
    fk = rms_norm(fk.reshape(B, S, N_DIFF_HEADS, 2, DIFF_QK_DIM), diff_k_gain)
    fv = fv.reshape(B, S, N_DIFF_HEADS, HEAD_DIM)
    lam_init = 0.8 - 0.6 * math.exp(-0.3 * layer)
    lam = (jnp.exp(jnp.sum(lam_q1.astype(F32) * lam_k1.astype(F32)))
           - jnp.exp(jnp.sum(lam_q2.astype(F32) * lam_k2.astype(F32))) + lam_init)
    diff = differential_attention(fq[..., 0, :], fq[..., 1, :], fk[..., 0, :], fk[..., 1, :],
                                  fv, slopes[N_DIL_HEADS:], lam)
    diff = rms_norm(diff, diff_out_gain) * (1.0 - lam_init)
    mixed = jnp.concatenate([dil_out, diff], axis=2).reshape(B, S, D_MODEL).astype(u.dtype)
    return mixed @ w_out


def chunk_gated_recurrence(q, k, v, log_f):
    B, S, H, K = q.shape
    V = v.shape[-1]
    C = HGRN_CHUNK
    n = S // C
    rs = lambda t: t.reshape(B, n, C, H, t.shape[-1]).transpose(1, 0, 3, 2, 4)
    q, k, v, lf = rs(q), rs(k), rs(v), rs(log_f)
    b = jnp.cumsum(lf, axis=-2)
    b_last = b[..., -1:, :]
    b_mid = b[..., C // 2:C // 2 + 1, :]
    scores = jnp.einsum('nbhtk,nbhsk->nbhts', q * jnp.exp(b - b_mid), k * jnp.exp(b_mid - b))
    causal = jnp.tril(jnp.ones((C, C), dtype=bool))
    o_intra = jnp.einsum('nbhts,nbhsv->nbhtv', jnp.where(causal, scores, 0.0), v)
    q_inter = q * jnp.exp(b)
    k_state = k * jnp.exp(b_last - b)
    chunk_decay = jnp.exp(b_last[..., 0, :])

    def step(state, xs):
        qi, ks, vc, dec = xs
        o = jnp.einsum('bhtk,bhkv->bhtv', qi, state)
        state = dec[..., None] * state + jnp.einsum('bhsk,bhsv->bhkv', ks, vc)
        return state, o

    _, o_inter = lax.scan(step, jnp.zeros((B, H, K, V), F32), (q_inter, k_state, v, chunk_decay))
    o = o_intra + o_inter
    return o.transpose(1, 0, 3, 2, 4).reshape(B, S, H, V)


def hgrn2_mixer(u, w_in, lb_logits, out_gain, w_out, layer):
    B, S, _ = u.shape
    q, f_logit, i, g = jnp.split(u @ w_in, [HGRN_FORGET_DIM, 2 * HGRN_FORGET_DIM,
                                            2 * HGRN_FORGET_DIM + D_MODEL], axis=-1)
    sm = jax.nn.softmax(lb_logits.astype(F32), axis=0)
    lb = (jnp.cumsum(sm, axis=0) - sm[0])[layer]
    fl = f_logit.astype(F32)
    log_f = jnp.logaddexp(jnp.log(lb), jnp.log1p(-lb) + jax.nn.log_sigmoid(fl))
    k = (1.0 - lb) * jax.nn.sigmoid(-fl)
    q = jax.nn.silu(q.astype(F32))
    heads = lambda t, d: t.reshape(B, S, HGRN_HEADS, d)
    o = chunk_gated_recurrence(heads(q, HGRN_EXPAND), heads(k, HGRN_EXPAND),
                               heads(i.astype(F32), HGRN_VDIM), heads(log_f, HGRN_EXPAND))
    o = rms_norm(o, out_gain) * jax.nn.sigmoid(heads(g.astype(F32), HGRN_VDIM))
    return o.reshape(B, S, D_MODEL).astype(u.dtype) @ w_out


def swiglu(u, w_gate, w_up, w_down):
    return (jax.nn.silu(u @ w_gate) * (u @ w_up)) @ w_down


def moe_swiglu(u, w_router, w_gate, w_up, w_down):
    B, S, D = u.shape
    t = u.reshape(B * S, D)
    logits = (t @ w_router).astype(F32)
    top_vals, top_idx = lax.top_k(logits, TOP_K)
    top_w = jax.nn.softmax(top_vals, axis=-1)
    gates = jnp.sum(jax.nn.one_hot(top_idx, N_EXPERTS, dtype=F32) * top_w[..., None], axis=1)
    out = jnp.zeros((B * S, D), F32)
    for e in range(N_EXPERTS):
        y = swiglu(t, w_gate[e], w_up[e], w_down[e]).astype(F32)
        out = out + gates[:, e:e + 1] * y
    return out.reshape(B, S, D).astype(u.dtype)


def setup_inputs(seed: int = 0) -> dict:
    key = jax.random.key(seed)
    ks = iter(jax.random.split(key, 40))
    nrm = lambda shape, scale: jax.random.normal(next(ks), shape, F32) * scale
    gain = lambda shape: 1.0 + nrm(shape, 0.02)
    D, E, O = D_MODEL, N_EVEN, N_ODD
    return {
        "x": nrm((BATCH, SEQ, D), 1.0),
        "attn_norm": gain((E, D)),
        "attn_w_in": nrm((E, D, ATTN_IN_WIDTH), D ** -0.5),
        "dil_q_gain": gain((E, HEAD_DIM)),
        "dil_k_gain": gain((E, HEAD_DIM)),
        "diff_q_gain": gain((E, DIFF_QK_DIM)),
        "diff_k_gain": gain((E, DIFF_QK_DIM)),
        "diff_lambda_q1": nrm((E, DIFF_QK_DIM), 0.1),
        "diff_lambda_k1": nrm((E, DIFF_QK_DIM), 0.1),
        "diff_lambda_q2": nrm((E, DIFF_QK_DIM), 0.1),
        "diff_lambda_k2": nrm((E, DIFF_QK_DIM), 0.1),
        "diff_out_gain": gain((E, HEAD_DIM)),
        "attn_w_out": nrm((E, D, D), D ** -0.5),
        "ffn_norm": gain((E, D)),
        "ffn_w_gate": nrm((E, D, D_FF), D ** -0.5),
        "ffn_w_up": nrm((E, D, D_FF), D ** -0.5),
        "ffn_w_down": nrm((E, D_FF, D), D_FF ** -0.5),
        "hgrn_norm": gain((O, D)),
        "hgrn_w_in": nrm((O, D, HGRN_IN_WIDTH), D ** -0.5),
        "hgrn_lb_logits": nrm((DEPTH, HGRN_FORGET_DIM), 0.5),
        "hgrn_out_gain": gain((O, HGRN_VDIM)),
        "hgrn_w_out": nrm((O, D, D), D ** -0.5),
        "moe_norm": gain((O, D)),
        "moe_w_router": nrm((O, D, N_EXPERTS), D ** -0.5),
        "moe_w_gate": nrm((O, N_EXPERTS, D, D_FF_EXPERT), D ** -0.5),
        "moe_w_up": nrm((O, N_EXPERTS, D, D_FF_EXPERT), D ** -0.5),
        "moe_w_down": nrm((O, N_EXPERTS, D_FF_EXPERT, D), D_FF_EXPERT ** -0.5),
    }


def reference(x, attn_norm, attn_w_in, dil_q_gain, dil_k_gain, diff_q_gain, diff_k_gain,
              diff_lambda_q1, diff_lambda_k1, diff_lambda_q2, diff_lambda_k2, diff_out_gain,
              attn_w_out, ffn_norm, ffn_w_gate, ffn_w_up, ffn_w_down,
              hgrn_norm, hgrn_w_in, hgrn_lb_logits, hgrn_out_gain, hgrn_w_out,
              moe_norm, moe_w_router, moe_w_gate, moe_w_up, moe_w_down):
    h = x
    for layer in range(DEPTH):
        j = layer // 2
        if layer % 2 == 0:
            h = h + dilated_diff_mixer(rms_norm(h, attn_norm[j]), attn_w_in[j], dil_q_gain[j], dil_k_gain[j],
                                       diff_q_gain[j], diff_k_gain[j], diff_lambda_q1[j], diff_lambda_k1[j],
                                       diff_lambda_q2[j], diff_lambda_k2[j], diff_out_gain[j],
                                       attn_w_out[j], layer).astype(h.dtype)
            h = h + swiglu(rms_norm(h, ffn_norm[j]), ffn_w_gate[j], ffn_w_up[j], ffn_w_down[j]).astype(h.dtype)
        else:
            h = h + hgrn2_mixer(rms_norm(h, hgrn_norm[j]), hgrn_w_in[j], hgrn_lb_logits,
                                hgrn_out_gain[j], hgrn_w_out[j], layer).astype(h.dtype)
            h = h + moe_swiglu(rms_norm(h, moe_norm[j]), moe_w_router[j], moe_w_gate[j],
                               moe_w_up[j], moe_w_down[j]).astype(h.dtype)
    return h
```

```python
import numpy as np
from contextlib import ExitStack
import concourse.bass as bass
import concourse.mybir as mybir
from concourse.bass_utils import run_bass_kernel_spmd

F32 = mybir.dt.float32
BF16 = mybir.dt.bfloat16
I32 = mybir.dt.int32
AF = mybir.ActivationFunctionType
ALU = mybir.AluOpType
AX = mybir.AxisListType

COMPUTE = ("pe", "act", "dve", "pool")
STRICT = True


class _Op:
    __slots__ = ("eng", "fn", "deps", "dma", "semkey", "val", "need_inc")


class Prog:
    def __init__(self, nc, tag="", fused=False):
        self.nc = nc
        self.tag = tag
        self.fused = fused
        self.ops = []
        self.last_w = {}
        self.readers = {}
        self.dma_cnt = {}
        self.es = ExitStack()

    def sb(self, name, shape, dt):
        return self.es.enter_context(self.nc.sbuf_tensor(self.tag + name, list(shape), dt))

    def ps(self, name, shape, dt=F32):
        return self.es.enter_context(self.nc.psum_tensor(self.tag + name, list(shape), dt))

    def add(self, eng, fn, reads=(), writes=(), dma=False, semkey=None):
        op = _Op()
        op.eng = eng
        op.fn = fn
        op.dma = dma
        op.need_inc = False
        op.val = None
        idx = len(self.ops)
        deps = {}
        for k in reads:
            w = self.last_w.get(k)
            if w is not None:
                deps[w] = True
        for k in writes:
            w = self.last_w.get(k)
            if w is not None:
                deps.setdefault(w, False)
            for r in self.readers.get(k, ()):
                deps.setdefault(r, False)
        op.deps = deps
        if dma:
            op.semkey = semkey if semkey is not None else (
                "dma:" + str(writes[0] if writes else reads[0]))
            self.dma_cnt[op.semkey] = self.dma_cnt.get(op.semkey, 0) + 1
            op.val = 16 * self.dma_cnt[op.semkey]
        self.ops.append(op)
        for k in writes:
            self.last_w[k] = idx
            self.readers[k] = []
        for k in reads:
            if k not in writes:
                self.readers.setdefault(k, []).append(idx)
        return idx

    def dma(self, out, in_, reads=(), writes=(), q="sp", semkey=None, **kw):
        def fn(e):
            return e.dma_start(out=out, in_=in_, **kw)
        return self.add(q, fn, reads, writes, dma=True, semkey=semkey)

    def emit(self):
        nc = self.nc
        ops = self.ops
        for i, op in enumerate(ops):
            for d, raw in op.deps.items():
                p = ops[d]
                if p.dma:
                    continue
                if p.eng == op.eng and not op.dma and (op.eng == "pe" or (not raw and not STRICT)):
                    continue
                p.need_inc = True
        cnt = {e: 0 for e in COMPUTE}
        for op in ops:
            if not op.dma and op.need_inc:
                cnt[op.eng] += 1
                op.val = cnt[op.eng]
        sems = {}
        def mksem(nm):
            if self.fused:
                return nc.alloc_semaphore(self.tag + nm)
            return self.es.enter_context(nc.semaphore(self.tag + nm))
        for e in COMPUTE:
            sems[e] = mksem("s_" + e)
        dkeys = sorted(self.dma_cnt.keys())
        for i, k in enumerate(dkeys):
            sems[k] = mksem("d%d" % i)
        engines = {}
        for i, op in enumerate(ops):
            engines.setdefault(op.eng, []).append(i)
        waited = {e: {} for e in engines}
        plan = {}
        for i, op in enumerate(ops):
            need = {}
            for d, raw in op.deps.items():
                p = ops[d]
                if p.dma:
                    key = p.semkey
                else:
                    if p.eng == op.eng and not op.dma and (op.eng == "pe" or (not raw and not STRICT)):
                        continue
                    key = p.eng
                v = p.val
                if v is None:
                    continue
                if need.get(key, 0) < v:
                    need[key] = v
            w = waited[op.eng]
            lst = []
            for key, v in need.items():
                if w.get(key, 0) < v:
                    w[key] = v
                    lst.append((key, v))
            plan[i] = lst
        final = [(k, 16 * n) for k, n in self.dma_cnt.items()]
        self.n_instr = len(ops)
        with nc.Block() as block:
            def run(engname, e):
                for i in engines.get(engname, ()):
                    op = ops[i]
                    for key, v in plan[i]:
                        e.wait_ge(sems[key], v)
                    ins = op.fn(e)
                    if op.dma:
                        ins.then_inc(sems[op.semkey], 16)
                    elif op.need_inc:
                        ins.then_inc(sems[op.eng], 1)
                if engname == "sp":
                    for k, v in final:
                        e.wait_ge(sems[k], v)

            @block.sync
            def _(e):
                run("sp", e)

            @block.tensor
            def _(e):
                run("pe", e)

            @block.scalar
            def _(e):
                run("act", e)

            @block.vector
            def _(e):
                run("dve", e)

            @block.gpsimd
            def _(e):
                run("pool", e)
        self.es.close()

import math

EPS = 1e-6
SLOPES = [2.0 ** (-8.0 * (h + 1) / 16) for h in range(16)]
DILS = [1, 4, 16]


def stt(e, out, in0, scalar, in1, op0=ALU.mult, op1=ALU.mult):
    return e.scalar_tensor_tensor(out=out, in0=in0, scalar=scalar, in1=in1, op0=op0, op1=op1)


class Ctx:
    def __init__(self, P, tabs_dram):
        self.P = P
        nc = P.nc
        self.tabs = P.sb("tabs_sb", [128, tabs_dram.shape[1]], F32)
        P.dma(self.tabs[:], tabs_dram, writes=["tabs"])
        self.ident = P.sb("ident", [128, 128], BF16)
        P.add("pool", lambda e: e.memset(self.ident[:], 0.0), writes=["ident"])
        P.add("pool", lambda e: e.affine_select(out=self.ident[:], in_=self.ident[:], pattern=[[-1, 128]],
                                                compare_op=ALU.not_equal, fill=1.0, base=0, channel_multiplier=1),
              reads=["ident"], writes=["ident"])
        self.ones = {}

    def ones_scaled(self, val, name):
        if name not in self.ones:
            t = self.P.sb("ones_" + name, [128, 128], BF16)
            self.P.add("pool", lambda e: e.memset(t[:], val), writes=["ones_" + name])
            self.ones[name] = t
        return self.ones[name]

    def blockdiag(self, blk, val, name):
        if name not in self.ones:
            t = self.P.sb("bd_" + name, [128, 128], BF16)
            key = "bd_" + name
            self.P.add("pool", lambda e: e.memset(t[:], val), writes=[key])
            for b in range(128 // blk):
                sl = t[:, b * blk:(b + 1) * blk]
                self.P.add("pool", lambda e, sl=sl, b=b: e.affine_select(out=sl, in_=sl, pattern=[[0, blk]],
                                                                       compare_op=ALU.is_ge, fill=0.0, base=-b * blk,
                                                                       channel_multiplier=1), reads=[key], writes=[key])
                self.P.add("pool", lambda e, sl=sl, b=b: e.affine_select(out=sl, in_=sl, pattern=[[0, blk]],
                                                                       compare_op=ALU.is_ge, fill=0.0,
                                                                       base=(b + 1) * blk - 1, channel_multiplier=-1),
                           reads=[key], writes=[key])
            self.ones[name] = t
        return self.ones[name]


def rms_tile(P, C, tag, xt, gcol0, xn_out, N, ps_key, ps, sq, sd, rstd, xn_key, x_key, xf_out=None):
    onesm = C.ones_scaled(1.0 / 1024, "d1024")
    _xa = xt(None); _xi = [xt(c) for c in range(8)]; _xo = [xn_out(c) for c in range(8)]
    _xf = [xf_out(c) for c in range(8)] if xf_out is not None else None
    xt = lambda c: _xa if c is None else _xi[c]
    xn_out = lambda c: _xo[c]
    if _xf is not None:
        xf_out = lambda c: _xf[c]
    P.add("act", lambda e: e.activation(out=sq[:, :, :N], in_=xt(None), func=AF.Square), reads=[x_key], writes=[tag + "sq"])
    for c in range(8):
        P.add("pe", lambda e, c=c: e.matmul(ps[:, :N], lhsT=onesm[:], rhs=sq[:, c, :N], start=(c == 0), stop=(c == 7)),
              reads=[tag + "sq", "ones_d1024"], writes=[ps_key])
    P.add("act", lambda e: e.activation(out=sd[:, :N], in_=ps[:, :N], func=AF.Sqrt, bias=EPS, scale=1.0),
          reads=[ps_key], writes=[tag + "sd"])
    P.add("dve", lambda e: e.reciprocal(out=rstd[:, :N], in_=sd[:, :N]), reads=[tag + "sd"], writes=[tag + "rstd"])
    for c in range(8):
        eng = "dve"
        if xf_out is not None:
            P.add(eng, lambda e, c=c: stt(e, xf_out(c), xt(c), C.tabs[:, gcol0 + c:gcol0 + c + 1], rstd[:, :N]),
                  reads=[x_key, tag + "rstd", "tabs"], writes=[xn_key + "f%d" % c])
            P.add("act", lambda e, c=c: e.activation(out=xn_out(c), in_=xf_out(c), func=AF.Copy),
                  reads=[xn_key + "f%d" % c], writes=[xn_key + "%d" % c])
        else:
            P.add(eng, lambda e, c=c: stt(e, xn_out(c), xt(c), C.tabs[:, gcol0 + c:gcol0 + c + 1], rstd[:, :N]),
                  reads=[x_key, tag + "rstd", "tabs"], writes=[xn_key + "%d" % c])


def load_w_hw(P, name, dram2d, K, N, slots, slot_keys, blk=512, keyfn=None, extra_slot_reads=()):
    kc = K // 128
    w = P.sb(name, [128, kc, N], BF16)
    if keyfn is None:
        keyfn = lambda c, col0: "%s_cb%d" % (name, col0 // 512)
    st = P.__dict__.setdefault("_slot_ctr", [0])
    engs = ("act", "dve", "pool")
    for col0 in range(0, N, blk):
        wd_ = min(blk, N - col0)
        for c in range(kc):
            si = st[0] % len(slots)
            st[0] += 1
            P.dma(slots[si][:, 0:wd_], dram2d[c * 128:(c + 1) * 128, col0:col0 + wd_], writes=[slot_keys[si]])
            eng = engs[st[0] % 3]
            dst = w[:, c, col0:col0 + wd_]
            src = slots[si][:, 0:wd_]
            if eng == "act":
                P.add("act", lambda e, dst=dst, src=src: e.activation(out=dst, in_=src, func=AF.Copy), reads=[slot_keys[si]], writes=[keyfn(c, col0)])
            else:
                P.add(eng, lambda e, dst=dst, src=src: e.tensor_copy(out=dst, in_=src), reads=[slot_keys[si]], writes=[keyfn(c, col0)])
    return w


T_ATTN_NORM = 0
T_FFN_NORM = 8
T_HGRN_NORM = 16
T_MOE_NORM = 24
T_DQG = 32
T_DKG = 33
T_FQG = 34
T_FKG = 35
T_DOG = 36
T_HOG = 37
T_PV = 38
T_M1 = 39
T_M2 = 40
T_LB0 = 41
T_LB1 = 49
T_LQ1 = 57
T_NT = 61


def phase_A(P, C, d):
    nc = P.nc
    xT = d["xT"]
    xTv = xT.rearrange("(c p) t -> p c t", p=128)
    xn1 = P.sb("A_xn", [128, 8, 2048], BF16)
    xt = P.sb("A_xt", [128, 8, 512], F32)
    xslot_keys = ["A_xt_s%d" % i for i in range(8)]
    w = load_w_hw(P, "A_w", d["attn_w_in"], 1024, 3072, [xt[:, i, :] for i in range(8)], xslot_keys,
                  keyfn=lambda c, col0: "A_w")
    sq = P.sb("A_sq", [128, 8, 512], BF16)
    sd = P.sb("A_sd", [128, 512], F32)
    rstd = P.sb("A_rstd", [128, 512], F32)
    sq2 = [P.sb("A_sq2_%d" % i, [128, 512], BF16) for i in range(2)]
    sd2 = [P.sb("A_sd2_%d" % i, [128, 512], F32) for i in range(2)]
    r2 = [P.sb("A_r2_%d" % i, [128, 512], F32) for i in range(2)]
    osts = [P.sb("A_ost%d" % i, [128, 14, 512], BF16) for i in range(2)]
    ost2 = P.sb("A_ost2", [128, 4, 2048], BF16)
    vst = P.sb("A_vst", [128, 12, 16, 65], BF16)
    vfst = P.sb("A_vfst", [128, 4, 16, 65], BF16)
    fac = P.sb("A_fac", [128, 4], F32)
    facv = P.sb("A_facv", [128, 4], F32)
    fac256 = P.sb("A_fac256", [128, 4, 64], F32)
    iop = P.sb("A_iop", [128, 1], F32)
    gq1 = P.sb("A_gq", [128, 2], F32)
    ps_ss = P.ps("A_ps_ss", [128, 512])
    pj = [P.ps("A_pj%d" % i, [128, 512]) for i in range(4)]
    ms = [P.ps("A_ms%d" % i, [128, 512]) for i in range(2)]
    bd64 = C.blockdiag(64, 1.0 / 64, "bd64")
    bd32 = C.blockdiag(32, 1.0 / 32, "bd32")
    tabs = C.tabs
    P.add("pool", lambda e: e.iota(iop[:], pattern=[[0, 1]], base=0, channel_multiplier=1,
                                   allow_small_or_imprecise_dtypes=True), writes=["A_iop"])
    for hh in range(4):
        P.add("act", lambda e, hh=hh: e.activation(out=fac[:, hh:hh + 1], in_=iop[:], func=AF.Exp, scale=SLOPES[12 + hh]),
              reads=["A_iop"], writes=["A_fac"])
    P.add("dve", lambda e: e.tensor_scalar(out=facv[:], in0=fac[:], scalar1=tabs[:, T_PV:T_PV + 1], scalar2=0.0,
                                           op0=ALU.mult, op1=ALU.add), reads=["A_fac", "tabs"], writes=["A_facv"])
    P.add("dve", lambda e: e.tensor_copy(out=fac256[:], in_=fac[:].unsqueeze(2).to_broadcast([128, 4, 64])),
          reads=["A_fac"], writes=["A_fac256"])
    P.add("dve", lambda e: e.tensor_tensor(out=gq1[:, 0:1], in0=tabs[:, T_FQG:T_FQG + 1], in1=tabs[:, T_M1:T_M1 + 1], op=ALU.mult),
          reads=["tabs"], writes=["A_gq"])
    P.add("dve", lambda e: e.tensor_tensor(out=gq1[:, 1:2], in0=tabs[:, T_FQG:T_FQG + 1], in1=tabs[:, T_M2:T_M2 + 1], op=ALU.mult),
          reads=["tabs"], writes=["A_gq"])

    cnt = {"pj": 0, "ms": 0, "tmp": 0}
    qtmp = [P.sb("A_qtmp%d" % i, [128, 512], F32) for i in range(2)]

    def qk_chunk(j, st, fc, bd, bdname, outs, xnb, col0, rr=None):
        jl_ = j % 4
        pi = cnt["pj"] % 4
        cnt["pj"] += 1
        mi = cnt["ms"] % 2
        cnt["ms"] += 1
        pk = "A_pj%d" % pi
        for c in range(8):
            P.add("pe", lambda e, c=c: e.matmul(pj[pi][:], lhsT=w[:, c, fc * 128:(fc + 1) * 128],
                                                rhs=xnb[:, c, col0:col0 + 512], start=(c == 0), stop=(c == 7)),
                  reads=["A_w", "A_xn_%d_%d" % (jl_, c)], writes=[pk])
        P.add("act", lambda e: e.activation(out=sq2[mi][:], in_=pj[pi][:], func=AF.Square), reads=[pk], writes=["A_sq2_%d" % mi])
        P.add("pe", lambda e: e.matmul(ms[mi][:], lhsT=bd[:], rhs=sq2[mi][:], start=True, stop=True),
              reads=["A_sq2_%d" % mi, "bd_" + bdname], writes=["A_ms%d" % mi])
        P.add("act", lambda e: e.activation(out=sd2[mi][:], in_=ms[mi][:], func=AF.Sqrt, bias=EPS, scale=1.0),
              reads=["A_ms%d" % mi], writes=["A_sd2_%d" % mi])
        P.add("dve", lambda e: e.reciprocal(out=r2[mi][:], in_=sd2[mi][:]), reads=["A_sd2_%d" % mi], writes=["A_r2_%d" % mi])
        def v3(ap):
            return ap if rr is None else ap.rearrange("p (u r) -> p u r", r=rr)
        for (g_ap, dst, dkey) in outs:
            ti = cnt["tmp"] % 2
            cnt["tmp"] += 1
            tmp = qtmp[ti]
            P.add("act", lambda e, g_ap=g_ap, tmp=tmp: e.activation(out=tmp[:], in_=pj[pi][:], func=AF.Copy, scale=g_ap),
                  reads=[pk, "tabs", "A_gq"], writes=["A_qtmp%d" % ti])
            P.add("pool", lambda e, dst=dst, tmp=tmp: e.tensor_tensor(out=dst, in0=v3(tmp[:]), in1=v3(r2[mi][:]), op=ALU.mult),
                  reads=["A_qtmp%d" % ti, "A_r2_%d" % mi], writes=[dkey])

    def perm_dst(buf_ap_full, g, jl):
        d_ = DILS[g]
        if g == 0:
            return buf_ap_full
        if g == 1:
            return buf_ap_full.rearrange("p (r u) -> p u r", r=4)
        return buf_ap_full.rearrange("p (r u) -> p u r", r=16)[:, jl * 32:(jl + 1) * 32, :]

    for st in range(4):
        xnb = xn1
        own = st >= 2
        need_dil = st >= 1
        for jl in range(4):
            j = st * 4 + jl
            ost = osts[j % 2]
            okey = "A_ost%d" % (j % 2)
            P.dma(xt[:], xTv[:, :, j * 512:(j + 1) * 512], writes=["A_xt"] + xslot_keys)
            col0 = jl * 512
            rms_tile(P, C, "A_", lambda c: (xt[:] if c is None else xt[:, c, :]), T_ATTN_NORM,
                     lambda c: xnb[:, c, col0:col0 + 512], 512, "A_ps_ss", ps_ss, sq, sd, rstd,
                     "A_xn_%d_" % jl, "A_xt")
            if own:
                for g in range(3):
                    for cc in range(2):
                        fc = g * 2 + cc
                        if g < 2:
                            dst = perm_dst(ost[:, g * 2 + cc, :], g, jl)
                            key = okey
                        else:
                            dst = perm_dst(ost2[:, cc, :], g, jl)
                            key = "A_ost2"
                        src_pj = None
                        qk_chunk(j, st, fc, bd64, "bd64", [(tabs[:, T_DQG:T_DQG + 1], dst, key)], xnb, col0, rr=(None, 4, 16)[g])
                for cc in range(2):
                    fc = 18 + cc
                    qk_chunk(j, st, fc, bd32, "bd32", [(gq1[:, 0:1], ost[:, 8 + cc, :], okey),
                                                       (gq1[:, 1:2], ost[:, 10 + cc, :], okey)], xnb, col0)
            if need_dil:
                for g in range(3):
                    for cc in range(2):
                        fc = 6 + g * 2 + cc
                        if g < 2:
                            dst = perm_dst(ost[:, 4 + g * 2 + cc, :], g, jl)
                            key = okey
                        else:
                            dst = perm_dst(ost2[:, 2 + cc, :], g, jl)
                            key = "A_ost2"
                        qk_chunk(j, st, fc, bd64, "bd64", [(tabs[:, T_DKG:T_DKG + 1], dst, key)], xnb, col0, rr=(None, 4, 16)[g])
            for cc in range(2):
                fc = 20 + cc
                qk_chunk(j, st, fc, bd32, "bd32", [(tabs[:, T_FKG:T_FKG + 1], ost[:, 12 + cc, :], okey)], xnb, col0)
            if own:
                t0 = (st - 2) * 2048 + jl * 512
                P.dma(d["qd"][0:512, t0:t0 + 512].rearrange("(c p) t -> p c t", p=128), ost[:, 0:4, :], reads=[okey])
                P.dma(d["qf1"][:, t0:t0 + 512].rearrange("(c p) t -> p c t", p=128), ost[:, 8:10, :], reads=[okey])
                P.dma(d["qf2"][:, t0:t0 + 512].rearrange("(c p) t -> p c t", p=128), ost[:, 10:12, :], reads=[okey])
            if need_dil:
                t1 = (st - 1) * 2048 + jl * 512
                P.dma(d["kd"][0:512, t1:t1 + 512].rearrange("(c p) t -> p c t", p=128), ost[:, 4:8, :], reads=[okey])
            P.dma(d["kf"][:, j * 512:(j + 1) * 512].rearrange("(c p) t -> p c t", p=128), ost[:, 12:14, :], reads=[okey])
        if own:
            t0 = (st - 2) * 2048
            P.dma(d["qd"][512:768, t0:t0 + 2048].rearrange("(c p) t -> p c t", p=128), ost2[:, 0:2, :], reads=["A_ost2"])
        if need_dil:
            t1 = (st - 1) * 2048
            P.dma(d["kd"][512:768, t1:t1 + 2048].rearrange("(c p) t -> p c t", p=128), ost2[:, 2:4, :], reads=["A_ost2"])
        xkeys = ["A_xn_%d_%d" % (q_, c) for c in range(8) for q_ in range(4)]
        if need_dil:
            if st == 1:
                P.add("pool", lambda e: e.tensor_copy(out=vst[:, :, :, 64:65], in_=tabs[:, T_PV:T_PV + 1].unsqueeze(1).unsqueeze(1).to_broadcast([128, 12, 16, 1])),
                      reads=["tabs"], writes=["A_vst"])
            elif st == 2:
                P.add("pool", lambda e: e.memset(vst[:, :, :, 64:65], 1.0), writes=["A_vst"])
            for g in range(3):
                d_ = DILS[g]
                nsp = 2048 // (128 * d_)
                for sp in range(nsp):
                    for r in range(d_):
                        blk = sp * d_ + r
                        pi = cnt["pj"] % 4
                        cnt["pj"] += 1
                        pk = "A_pj%d" % pi
                        s0 = sp * 128 * d_ + r
                        for c in range(8):
                            P.add("pe", lambda e, c=c, s0=s0, d_=d_, g=g, pi=pi: e.matmul(
                                pj[pi][:, 0:256], lhsT=xnb[:, c, s0:s0 + 127 * d_ + 1:d_],
                                rhs=w[:, c, 1536 + g * 256:1536 + (g + 1) * 256], start=(c == 0), stop=(c == 7)),
                                reads=["A_w"] + xkeys, writes=[pk])
                        eng = "act" if blk % 2 == 0 else "dve"
                        if eng == "act":
                            P.add("act", lambda e, g=g, blk=blk, pi=pi: e.activation(
                                out=vst[:, g * 4:(g + 1) * 4, blk, 0:64],
                                in_=pj[pi][:, 0:256].rearrange("p (h f) -> p h f", h=4), func=AF.Copy),
                                reads=[pk], writes=["A_vst"])
                        else:
                            P.add("dve", lambda e, g=g, blk=blk, pi=pi: e.tensor_copy(
                                out=vst[:, g * 4:(g + 1) * 4, blk, 0:64],
                                in_=pj[pi][:, 0:256].rearrange("p (h f) -> p h f", h=4)),
                                reads=[pk], writes=["A_vst"])
            P.dma(d["vd"][:, :, (st - 1) * 16:st * 16, :].rearrange("h p b f -> p h b f"), vst[:], reads=["A_vst"])
        src_f = facv if st < 2 else fac
        P.add("pool", lambda e, src_f=src_f: e.tensor_copy(out=vfst[:, :, :, 64:65],
                                                          in_=src_f[:].unsqueeze(2).unsqueeze(3).to_broadcast([128, 4, 16, 1])),
              reads=["A_fac", "A_facv"], writes=["A_vfst"])
        for bk in range(16):
            pi = cnt["pj"] % 4
            cnt["pj"] += 1
            pk = "A_pj%d" % pi
            for c in range(8):
                P.add("pe", lambda e, c=c, bk=bk, pi=pi: e.matmul(pj[pi][:, 0:256], lhsT=xnb[:, c, bk * 128:(bk + 1) * 128],
                                                                   rhs=w[:, c, 2816:3072], start=(c == 0), stop=(c == 7)),
                      reads=["A_w"] + xkeys, writes=[pk])
            P.add("dve", lambda e, bk=bk, pi=pi: e.tensor_tensor(out=vfst[:, :, bk, 0:64],
                                                                in0=pj[pi][:, 0:256].rearrange("p (h f) -> p h f", h=4),
                                                                in1=fac256[:], op=ALU.mult),
                  reads=[pk, "A_fac256"], writes=["A_vfst"])
        P.dma(d["vf"][:, :, st * 16:(st + 1) * 16, :].rearrange("h p b f -> p h b f"), vfst[:], reads=["A_vfst"])


def phase_B(P, C, d):
    tabs = C.tabs
    mixT = d["mixT"]
    onesf = P.sb("B_onesf", [128, 128], F32)
    P.add("pool", lambda e: e.memset(onesf[:], 1.0), writes=["B_onesf"])
    ones64 = C.blockdiag(64, 1.0 / 64, "bd64")
    Mt = P.sb("B_M", [128, 12, 256], BF16)
    stp = P.sb("B_stp", [128, 256], F32)
    mtmp = P.sb("B_mtmp", [128, 256], F32)
    P.add("pool", lambda e: e.iota(stp[:, 0:128], pattern=[[1, 128]], base=128, channel_multiplier=-1,
                                   allow_small_or_imprecise_dtypes=True), writes=["B_stp"])
    P.add("pool", lambda e: e.iota(stp[:, 128:256], pattern=[[1, 128]], base=0, channel_multiplier=-1,
                                   allow_small_or_imprecise_dtypes=True), writes=["B_stp"])
    for h in range(12):
        sc = -SLOPES[h] * DILS[h // 4]
        P.add("dve", lambda e: e.tensor_scalar(out=mtmp[:], in0=stp[:], scalar1=0.0, scalar2=0.0, op0=ALU.max, op1=ALU.add),
              reads=["B_stp"], writes=["B_mtmp"])
        P.add("act", lambda e, sc=sc: e.activation(out=mtmp[:], in_=mtmp[:], func=AF.Exp, scale=sc), reads=["B_mtmp"], writes=["B_mtmp"])
        P.add("pool", lambda e: e.affine_select(out=mtmp[:, 0:128], in_=mtmp[:, 0:128], pattern=[[-1, 128]], compare_op=ALU.is_ge,
                                                fill=0.0, base=0, channel_multiplier=1), reads=["B_mtmp"], writes=["B_mtmp"])
        P.add("pool", lambda e: e.affine_select(out=mtmp[:, 128:256], in_=mtmp[:, 128:256], pattern=[[1, 128]], compare_op=ALU.is_ge,
                                                fill=0.0, base=0, channel_multiplier=-1), reads=["B_mtmp"], writes=["B_mtmp"])
        P.add("dve", lambda e, h=h: e.tensor_copy(out=Mt[:, h, :], in_=mtmp[:]), reads=["B_mtmp"], writes=["B_M"])
    cm = P.sb("B_cm", [128, 4, 512], BF16)
    P.add("pool", lambda e: e.memset(cm[:], 1.0), writes=["B_cm"])
    for i in range(4):
        P.add("pool", lambda e, i=i: e.affine_select(out=cm[:, i, :], in_=cm[:, i, :], pattern=[[1, 512]], compare_op=ALU.is_ge,
                                                     fill=0.0, base=-128 * i, channel_multiplier=-1), reads=["B_cm"], writes=["B_cm"])
    lp = P.sb("B_lp", [128, 2], F32)
    P.add("pool", lambda e: e.memset(lp[:], 0.0), writes=["B_lp"])
    P.add("dve", lambda e: e.tensor_tensor(out=lp[0:32, 0:1], in0=tabs[0:32, T_LQ1:T_LQ1 + 1], in1=tabs[0:32, T_LQ1 + 1:T_LQ1 + 2], op=ALU.mult),
          reads=["tabs", "B_lp"], writes=["B_lp"])
    P.add("dve", lambda e: e.tensor_tensor(out=lp[0:32, 1:2], in0=tabs[0:32, T_LQ1 + 2:T_LQ1 + 3], in1=tabs[0:32, T_LQ1 + 3:T_LQ1 + 4], op=ALU.mult),
          reads=["tabs", "B_lp"], writes=["B_lp"])
    bank = [P.ps("B_bank%d" % i, [128, 1024]) for i in range(4)]
    bk = ["B_bank%d" % i for i in range(4)]
    P.add("pe", lambda e: e.matmul(bank[0][:, 0:2], lhsT=onesf[:, :], rhs=lp[:, :], start=True, stop=True),
          reads=["B_onesf", "B_lp"], writes=[bk[0]])
    le = P.sb("B_le", [128, 2], F32)
    neglam = P.sb("B_neglam", [128, 1], F32)
    g08 = P.sb("B_g08", [128, 1], F32)
    P.add("act", lambda e: e.activation(out=le[:], in_=bank[0][:, 0:2], func=AF.Exp), reads=[bk[0]], writes=["B_le"])
    P.add("dve", lambda e: e.tensor_tensor(out=neglam[:], in0=le[:, 1:2], in1=le[:, 0:1], op=ALU.subtract), reads=["B_le"], writes=["B_neglam"])
    P.add("dve", lambda e: e.tensor_scalar(out=neglam[:], in0=neglam[:], scalar1=-0.2, scalar2=0.0, op0=ALU.add, op1=ALU.add),
          reads=["B_neglam"], writes=["B_neglam"])
    P.add("dve", lambda e: e.tensor_scalar(out=g08[:], in0=tabs[:, T_DOG:T_DOG + 1], scalar1=0.8, scalar2=0.0, op0=ALU.mult, op1=ALU.add),
          reads=["tabs"], writes=["B_g08"])

    qb = [P.sb("B_q%d" % i, [128, 3, 2048], BF16) for i in range(1)]
    kb_ = [P.sb("B_k%d" % i, [128, 3, 4096], BF16) for i in range(1)]
    vb = [P.sb("B_v%d" % i, [128, 3, 32, 65], BF16) for i in range(1)]
    nd = P.sb("B_nd", [65, 3, 2048], F32)
    Eb = [P.sb("B_E%d" % i, [128, 256], BF16) for i in range(3)]
    Pb = [P.sb("B_P%d" % i, [128, 256], BF16) for i in range(3)]
    rinv = P.sb("B_rinv", [64, 512], F32)
    mixst = P.sb("B_mixst", [64, 3, 2048], BF16)
    unit = 0
    cnt = 0
    for hp in range(4):
        for S in range(2):
            bi = 0
            unit += 1
            hf_ = (hp % 2) * 64
            P.add("pool", lambda e, bi=bi: e.memset(qb[bi][:], 0.0), writes=["B_q%d_%d" % (bi, g) for g in range(3)])
            for g in range(3):
                r0 = (4 * g + hp) * 64
                rp = (4 * g + (hp // 2) * 2) * 64
                P.dma(qb[bi][hf_:hf_ + 64, g, :], d["qd"][r0:r0 + 64, S * 2048:(S + 1) * 2048], writes=["B_q%d_%d" % (bi, g)])
                P.dma(kb_[bi][:, g, :], d["kd"][rp:rp + 128, S * 2048:S * 2048 + 4096], writes=["B_k%d_%d" % (bi, g)])
                P.dma(vb[bi][:, g, :, :], d["vd"][4 * g + hp, :, S * 16:S * 16 + 32, :], writes=["B_v%d_%d" % (bi, g)])
            items = []
            for g in range(3):
                dd = DILS[g]
                h = 4 * g + hp
                for blk in range(16):
                    sp, r = blk // dd, blk % dd
                    pb_, cb_ = 16 + blk - dd, 16 + blk
                    ps = bank[cnt % 2]
                    psk = bk[cnt % 2]
                    E = Eb[cnt % 3]
                    Pm = Pb[cnt % 3]
                    ek, pk_ = "B_E%d" % (cnt % 3), "B_P%d" % (cnt % 3)
                    po = bank[2 + cnt % 2]
                    pok = bk[2 + cnt % 2]
                    cnt += 1
                    par = cnt % 2
                    qs = qb[bi][:, g, blk * 128:(blk + 1) * 128]

                    def st1(ps=ps, psk=psk, g=g, pb_=pb_, cb_=cb_, qs=qs, bi=bi):
                        P.add("pe", lambda e: e.matmul(ps[:, 0:128], lhsT=kb_[bi][:, g, pb_ * 128:(pb_ + 1) * 128], rhs=qs, start=True, stop=True),
                              reads=["B_q%d_%d" % (bi, g), "B_k%d_%d" % (bi, g)], writes=[psk])
                        P.add("pe", lambda e: e.matmul(ps[:, 128:256], lhsT=kb_[bi][:, g, cb_ * 128:(cb_ + 1) * 128], rhs=qs, start=True, stop=True),
                              reads=["B_q%d_%d" % (bi, g), "B_k%d_%d" % (bi, g)], writes=[psk])

                    def st2(ps=ps, psk=psk, E=E, Pm=Pm, ek=ek, pk_=pk_, h=h, par=par):
                        P.add("act", lambda e: e.activation(out=E[:], in_=ps[:, 0:256], func=AF.Exp, scale=0.125), reads=[psk], writes=[ek])
                        eng = "pool" if par == 0 else "dve"
                        P.add(eng, lambda e: e.tensor_tensor(out=Pm[:], in0=E[:], in1=Mt[:, h, :], op=ALU.mult), reads=[ek, "B_M"], writes=[pk_])

                    def st3(po=po, pok=pok, Pm=Pm, pk_=pk_, g=g, pb_=pb_, cb_=cb_, bi=bi, sp=sp, r=r, dd=dd, par=par):
                        P.add("pe", lambda e: e.matmul(po[0:65, 0:128], lhsT=vb[bi][:, g, pb_, :], rhs=Pm[:, 0:128], start=True, stop=False),
                              reads=[pk_, "B_v%d_%d" % (bi, g)], writes=[pok])
                        P.add("pe", lambda e: e.matmul(po[0:65, 0:128], lhsT=vb[bi][:, g, cb_, :], rhs=Pm[:, 128:256], start=False, stop=True),
                              reads=[pk_, "B_v%d_%d" % (bi, g)], writes=[pok])
                        t0 = sp * 128 * dd + r
                        dst = nd[:, g, t0:t0 + 127 * dd + 1:dd]
                        if par == 0:
                            P.add("act", lambda e: e.activation(out=dst, in_=po[0:65, 0:128], func=AF.Copy), reads=[pok], writes=["B_nd%d" % g])
                        else:
                            P.add("dve", lambda e: e.tensor_copy(out=dst, in_=po[0:65, 0:128]), reads=[pok], writes=["B_nd%d" % g])
                    items.append((st1, st2, st3))
            items[0][0]()
            for ii in range(len(items)):
                if ii + 1 < len(items):
                    items[ii + 1][0]()
                items[ii][1]()
                items[ii][2]()
            for cc in range(4):
                pd = bank[cnt % 2]
                pdk = bk[cnt % 2]
                cnt += 1
                for g in range(3):
                    P.add("pe", lambda e, pd=pd, g=g, cc=cc: e.matmul(pd[0:64, 0:512], lhsT=onesf[64:65, 0:64], rhs=nd[64:65, g, cc * 512:(cc + 1) * 512],
                                                                     start=(g == 0), stop=(g == 2)), reads=["B_onesf", "B_nd%d" % g], writes=[pdk])
                P.add("dve", lambda e, pd=pd: e.reciprocal(out=rinv[:], in_=pd[0:64, 0:512]), reads=[pdk], writes=["B_rinv"])
                for g in range(3):
                    eng = "pool" if g == 1 else "dve"
                    P.add(eng, lambda e, g=g, cc=cc: e.tensor_tensor(out=mixst[:, g, cc * 512:(cc + 1) * 512], in0=nd[0:64, g, cc * 512:(cc + 1) * 512],
                                                                    in1=rinv[:], op=ALU.mult), reads=["B_nd%d" % g, "B_rinv"], writes=["B_mixst%d" % g])
            for g in range(3):
                r0 = (4 * g + hp) * 64
                P.dma(mixT[r0:r0 + 64, S * 2048:(S + 1) * 2048], mixst[:, g, :], reads=["B_mixst%d" % g])

    kf_ = P.sb("B_kf", [128, 8192], BF16)
    vf_ = P.sb("B_vf", [128, 64, 65], BF16)
    q12 = P.sb("B_q12", [128, 2, 4096], BF16)
    E2 = [P.sb("B_E2_%d" % i, [128, 1024], BF16) for i in range(3)]
    dn = P.sb("B_dn", [65, 1024], F32)
    rb = P.sb("B_rb", [64, 1024], F32)
    t1 = P.sb("B_t1", [64, 512], F32)
    t2 = P.sb("B_t2", [64, 512], F32)
    sqd = P.sb("B_sqd", [64, 512], BF16)
    sdd = P.sb("B_sdd", [64, 512], F32)
    mixf = [P.sb("B_mixf%d" % i, [64, 512], BF16) for i in range(2)]
    sc32 = 32.0 ** -0.5
    ec = 0
    for h in range(4):
        sl = SLOPES[12 + h]
        hf_ = (h % 2) * 64
        rp = (h // 2) * 128
        P.dma(kf_[:], d["kf"][rp:rp + 128, :], writes=["B_kf"])
        P.add("pool", lambda e: e.memset(q12[:], 0.0), writes=["B_q12"])
        P.dma(vf_[:], d["vf"][h], writes=["B_vf"])
        P.dma(q12[hf_:hf_ + 64, 0, :], d["qf1"][h * 64:(h + 1) * 64, :], writes=["B_q12"])
        P.dma(q12[hf_:hf_ + 64, 1, :], d["qf2"][h * 64:(h + 1) * 64, :], writes=["B_q12"])
        for jj in range(8):
            nkb = 32 + 4 * jj + 4
            po = bank[2]
            pok = bk[2]
            items = []
            for kb in range(nkb):
                ps = bank[kb % 2]
                psk = bk[kb % 2]
                E = E2[ec % 3]
                ek = "B_E2_%d" % (ec % 3)
                ec += 1
                off = -sl * (4096 + jj * 512 - kb * 128)
                di = kb - (32 + 4 * jj)

                def st1(ps=ps, psk=psk, kb=kb, jj=jj):
                    for m in range(2):
                        P.add("pe", lambda e, m=m: e.matmul(ps[:, m * 512:(m + 1) * 512], lhsT=kf_[:, kb * 128:(kb + 1) * 128],
                                                            rhs=q12[:, m, jj * 512:(jj + 1) * 512], start=True, stop=True),
                              reads=["B_kf", "B_q12"], writes=[psk])

                def st2(ps=ps, psk=psk, E=E, ek=ek, off=off, di=di):
                    P.add("act", lambda e: e.activation(out=E[:], in_=ps[:], func=AF.Exp, bias=off, scale=sc32), reads=[psk], writes=[ek])
                    if di >= 0:
                        eng = "pool" if di % 2 == 0 else "dve"
                        P.add(eng, lambda e: e.tensor_tensor(out=E[:].rearrange("p (m q) -> p m q", m=2), in0=E[:].rearrange("p (m q) -> p m q", m=2),
                                                             in1=cm[:, di, :].unsqueeze(1).to_broadcast([128, 2, 512]), op=ALU.mult),
                              reads=[ek, "B_cm"], writes=[ek])

                def st3(E=E, ek=ek, kb=kb, nkb=nkb):
                    for m in range(2):
                        P.add("pe", lambda e, m=m: e.matmul(po[0:65, m * 512:(m + 1) * 512], lhsT=vf_[:, kb, :], rhs=E[:, m * 512:(m + 1) * 512],
                                                            start=(kb == 0), stop=(kb == nkb - 1)), reads=[ek, "B_vf"], writes=[pok])
                items.append((st1, st2, st3))
            items[0][0]()
            for ii in range(len(items)):
                if ii + 1 < len(items):
                    items[ii + 1][0]()
                items[ii][1]()
                items[ii][2]()
            P.add("act", lambda e: e.activation(out=dn[64:65, :], in_=po[64:65, :], func=AF.Copy), reads=[pok], writes=["B_dn"])
            P.add("dve", lambda e: e.reciprocal(out=dn[64:65, :], in_=dn[64:65, :]), reads=["B_dn"], writes=["B_dn"])
            pbb = bank[3]
            for m in range(2):
                P.add("pe", lambda e, m=m: e.matmul(pbb[0:64, m * 512:(m + 1) * 512], lhsT=onesf[64:65, 0:64], rhs=dn[64:65, m * 512:(m + 1) * 512], start=True, stop=True),
                      reads=["B_dn", "B_onesf"], writes=[bk[3]])
            P.add("act", lambda e: e.activation(out=rb[:], in_=pbb[0:64, :], func=AF.Copy), reads=[bk[3]], writes=["B_rb"])
            P.add("dve", lambda e: e.tensor_tensor(out=t1[:], in0=po[0:64, 0:512], in1=rb[:, 0:512], op=ALU.mult), reads=[pok, "B_rb"], writes=["B_t1"])
            P.add("dve", lambda e: stt(e, t2[:], po[0:64, 512:1024], neglam[0:64, 0:1], rb[:, 512:1024]), reads=[pok, "B_rb", "B_neglam"], writes=["B_t2"])
            P.add("pool", lambda e: e.tensor_tensor(out=t1[:], in0=t1[:], in1=t2[:], op=ALU.add), reads=["B_t1", "B_t2"], writes=["B_t1"])
            P.add("act", lambda e: e.activation(out=sqd[:], in_=t1[:], func=AF.Square), reads=["B_t1"], writes=["B_sqd"])
            P.add("pe", lambda e: e.matmul(pbb[0:64, 0:512], lhsT=ones64[0:64, 0:64], rhs=sqd[:], start=True, stop=True), reads=["B_sqd", "bd_bd64"], writes=[bk[3]])
            P.add("act", lambda e: e.activation(out=sdd[:], in_=pbb[0:64, 0:512], func=AF.Sqrt, bias=EPS, scale=1.0), reads=[bk[3]], writes=["B_sdd"])
            P.add("dve", lambda e: e.reciprocal(out=sdd[:], in_=sdd[:]), reads=["B_sdd"], writes=["B_sdd"])
            mf = mixf[jj % 2]
            mk = "B_mixf%d" % (jj % 2)
            P.add("dve", lambda e, mf=mf: stt(e, mf[:], t1[:], g08[0:64, 0:1], sdd[:]), reads=["B_t1", "B_sdd", "B_g08"], writes=[mk])
            r0 = 768 + h * 64
            P.dma(mixT[r0:r0 + 64, jj * 512:(jj + 1) * 512], mf[:], reads=[mk])


def load_w_bf16(P, name, dram2d, K, N):
    kc = K // 128
    w = P.sb(name, [128, kc, N], BF16)
    for c in range(kc):
        P.dma(w[:, c, :], dram2d[c * 128:(c + 1) * 128, :], writes=[name], q="pool")
    return w


def phase_C(P, C, d):
    tabs = C.tabs
    N = 256
    stg = P.sb("C_stg", [128, 4, 512], F32)
    slots = [stg[:, i, :] for i in range(4)]
    skeys = ["C_stg%d" % i for i in range(4)]
    wout = load_w_hw(P, "C_wout", d["attn_w_out"], 1024, 1024, slots, skeys)
    wg = load_w_hw(P, "C_wg", d["ffn_w_gate"], 1024, 2816, slots, skeys)
    wu = load_w_hw(P, "C_wu", d["ffn_w_up"], 1024, 2816, slots, skeys)
    wd = load_w_hw(P, "C_wd", d["ffn_w_down"], 2816, 1024, slots, skeys)
    mix = [P.sb("C_mix%d" % i, [128, 8, N], BF16) for i in range(2)]
    xh = [P.sb("C_xh%d" % i, [128, 8, N], F32) for i in range(2)]
    sq = P.sb("C_sq", [128, 8, N], BF16)
    sd = P.sb("C_sd", [128, N], F32)
    rstd = P.sb("C_rstd", [128, N], F32)
    xn = P.sb("C_xn", [128, 8, N], BF16)
    a = P.sb("C_a", [128, 22, N], BF16)
    sg = [P.sb("C_sg%d" % i, [128, N], F32) for i in range(2)]
    ps_ss = P.ps("C_ps_ss", [128, 512])
    pb = [P.ps("C_pb%d" % i, [128, 512]) for i in range(6)]
    xTv = d["xT"].rearrange("(c p) t -> p c t", p=128)
    mTv = d["mixT"].rearrange("(c p) t -> p c t", p=128)
    hTv = d["h2T"].rearrange("(c p) t -> p c t", p=128)
    pc = 0
    for j in range(4096 // N):
        b = j % 2
        P.dma(mix[b][:], mTv[:, :, j * N:(j + 1) * N], writes=["C_mix%d" % b])
        P.dma(xh[b][:], xTv[:, :, 4096 + j * N:4096 + (j + 1) * N], writes=["C_xh%d_%d" % (b, c) for c in range(8)])
        for oc in range(8):
            pp = pb[pc % 6]; pk = "C_pb%d" % (pc % 6); pc += 1
            for c in range(8):
                P.add("pe", lambda e, pp=pp, c=c, oc=oc, b=b: e.matmul(pp[:, 0:N], lhsT=wout[:, c, oc * 128:(oc + 1) * 128], rhs=mix[b][:, c, :],
                                                                       start=(c == 0), stop=(c == 7)), reads=["C_wout_cb%d" % (oc // 4), "C_mix%d" % b], writes=[pk])
            P.add("dve", lambda e, pp=pp, oc=oc, b=b: e.tensor_tensor(out=xh[b][:, oc, :], in0=pp[:, 0:N], in1=xh[b][:, oc, :], op=ALU.add),
                  reads=[pk, "C_xh%d_%d" % (b, oc)], writes=["C_xh%d_%d" % (b, oc)])
        xkeys = ["C_xh%d_%d" % (b, c) for c in range(8)]
        onesm = C.ones_scaled(1.0 / 1024, "d1024")
        P.add("act", lambda e, b=b: e.activation(out=sq[:], in_=xh[b][:], func=AF.Square), reads=xkeys, writes=["C_sq"])
        for c in range(8):
            P.add("pe", lambda e, c=c: e.matmul(ps_ss[:, 0:N], lhsT=onesm[:], rhs=sq[:, c, :], start=(c == 0), stop=(c == 7)),
                  reads=["C_sq", "ones_d1024"], writes=["C_ps_ss"])
        P.add("act", lambda e: e.activation(out=sd[:], in_=ps_ss[:, 0:N], func=AF.Sqrt, bias=EPS, scale=1.0), reads=["C_ps_ss"], writes=["C_sd"])
        P.add("dve", lambda e: e.reciprocal(out=rstd[:], in_=sd[:]), reads=["C_sd"], writes=["C_rstd"])
        for c in range(8):
            P.add("dve", lambda e, c=c, b=b: stt(e, xn[:, c, :], xh[b][:, c, :], tabs[:, T_FFN_NORM + c:T_FFN_NORM + c + 1], rstd[:]),
                  reads=["C_xh%d_%d" % (b, c), "C_rstd", "tabs"], writes=["C_xn%d" % c])
        nkeys = ["C_xn%d" % c for c in range(8)]
        for f in range(22):
            pg = pb[pc % 6]; pgk = "C_pb%d" % (pc % 6); pc += 1
            pu = pb[pc % 6]; puk = "C_pb%d" % (pc % 6); pc += 1
            for c in range(8):
                P.add("pe", lambda e, pg=pg, c=c, f=f: e.matmul(pg[:, 0:N], lhsT=wg[:, c, f * 128:(f + 1) * 128], rhs=xn[:, c, :], start=(c == 0), stop=(c == 7)),
                      reads=["C_wg_cb%d" % (f // 4)] + nkeys, writes=[pgk])
            for c in range(8):
                P.add("pe", lambda e, pu=pu, c=c, f=f: e.matmul(pu[:, 0:N], lhsT=wu[:, c, f * 128:(f + 1) * 128], rhs=xn[:, c, :], start=(c == 0), stop=(c == 7)),
                      reads=["C_wu_cb%d" % (f // 4)] + nkeys, writes=[puk])
            s_ = sg[f % 2]; sk = "C_sg%d" % (f % 2)
            P.add("act", lambda e, pg=pg, s_=s_: e.activation(out=s_[:], in_=pg[:, 0:N], func=AF.Silu), reads=[pgk], writes=[sk])
            P.add("dve", lambda e, pu=pu, s_=s_, f=f: e.tensor_tensor(out=a[:, f, :], in0=pu[:, 0:N], in1=s_[:], op=ALU.mult), reads=[puk, sk], writes=["C_a%d" % f])
        akeys = ["C_a%d" % f for f in range(22)]
        for oc in range(8):
            pp = pb[pc % 6]; pk = "C_pb%d" % (pc % 6); pc += 1
            for f in range(22):
                P.add("pe", lambda e, pp=pp, f=f, oc=oc: e.matmul(pp[:, 0:N], lhsT=wd[:, f, oc * 128:(oc + 1) * 128], rhs=a[:, f, :], start=(f == 0), stop=(f == 21)),
                      reads=["C_wd_cb%d" % (oc // 4)] + akeys, writes=[pk])
            P.add("dve", lambda e, pp=pp, oc=oc, b=b: e.tensor_tensor(out=xh[b][:, oc, :], in0=pp[:, 0:N], in1=xh[b][:, oc, :], op=ALU.add),
                  reads=[pk, "C_xh%d_%d" % (b, oc)], writes=["C_xh%d_%d" % (b, oc)])
        P.dma(hTv[:, :, j * N:(j + 1) * N], xh[b][:], reads=xkeys)


def phase_D(P, C, d, zero_init=False, scale_pv=False, write_h=True, state_only=False):
    tabs = C.tabs
    ident = C.ident
    N = 512
    full = not state_only
    ht = P.sb("D_ht", [128, 8, N], F32)
    hslots = [ht[:, i, :] for i in range(8)]
    hslot_keys = ["D_ht%d" % i for i in range(8)]
    if full:
        w = load_w_hw(P, "D_w", d["hgrn_w_in"], 1024, 4096, hslots, hslot_keys, keyfn=lambda c, col0: "D_w")
        wo = load_w_hw(P, "D_wo", d["hgrn_w_out"], 1024, 1024, hslots, hslot_keys, keyfn=lambda c, col0: "D_wo")
    else:
        w = P.sb("D_w", [128, 8, 4096], BF16)
        wpart = load_w_hw(P, "D_wfi", d["hgrn_w_in"][:, 1024:3072], 1024, 2048, hslots, hslot_keys, keyfn=lambda c, col0: "D_w")
        w_full = w
        class _W:
            def __getitem__(self, key):
                p_, c_, cols = key
                return wpart[p_, c_, slice(cols.start - 1024, cols.stop - 1024)]
        w = _W()
    onesm = C.ones_scaled(1.0 / 1024, "d1024")
    ones128 = C.ones_scaled(1.0 / 128, "d128")
    lb = P.sb("D_lb", [128, 8], F32)
    oml = P.sb("D_oml", [128, 8], F32)
    P.add("dve", lambda e: e.tensor_tensor(out=lb[:], in0=tabs[:, T_LB0:T_LB0 + 8], in1=tabs[:, T_LB1:T_LB1 + 8], op=ALU.subtract), reads=["tabs"], writes=["D_lb"])
    P.add("act", lambda e: e.activation(out=lb[:], in_=lb[:], func=AF.Exp), reads=["D_lb"], writes=["D_lb"])
    P.add("dve", lambda e: e.tensor_scalar(out=lb[:], in0=lb[:], scalar1=1.0, scalar2=0.0, op0=ALU.add, op1=ALU.add), reads=["D_lb"], writes=["D_lb"])
    P.add("dve", lambda e: e.reciprocal(out=lb[:], in_=lb[:]), reads=["D_lb"], writes=["D_lb"])
    P.add("dve", lambda e: e.tensor_scalar(out=oml[:], in0=lb[:], scalar1=-1.0, scalar2=1.0, op0=ALU.mult, op1=ALU.add), reads=["D_lb"], writes=["D_oml"])
    noml = P.sb("D_noml", [128, 8], F32)
    P.add("dve", lambda e: e.tensor_scalar(out=noml[:], in0=oml[:], scalar1=-1.0, scalar2=0.0, op0=ALU.mult, op1=ALU.add), reads=["D_oml"], writes=["D_noml"])
    m2 = P.sb("D_m2", [128, 128], F32)
    P.add("pool", lambda e: e.memset(m2[:], 1.0), writes=["D_m2"])
    P.add("pool", lambda e: e.affine_select(out=m2[:], in_=m2[:], pattern=[[1, 128]], compare_op=ALU.is_ge, fill=0.0, base=0, channel_multiplier=-1),
          reads=["D_m2"], writes=["D_m2"])
    P.add("pool", lambda e: e.memset(m2[0:64, 64:128], 0.0), reads=["D_m2"], writes=["D_m2"])
    rm = P.sb("D_rm", [128, N], F32)
    P.add("pool", lambda e: e.memset(rm[:], 1.0), writes=["D_rm"])
    P.add("pool", lambda e: e.memset(rm[:].rearrange("p (a b) -> p a b", b=64)[:, :, 0:1], 0.0), reads=["D_rm"], writes=["D_rm"])
    stf = P.sb("D_stf", [128, 8, 128], F32)
    stb = P.sb("D_stb", [128, 8, 128], BF16)
    skeys = ["D_stf%d" % h for h in range(8)]
    if zero_init:
        P.add("pool", lambda e: e.memset(stf[:], 0.0), writes=skeys)
    else:
        P.dma(stf[:], d["s_in"].rearrange("h k v -> k h v"), writes=skeys)
        if scale_pv:
            P.add("dve", lambda e: e.tensor_scalar(out=stf[:], in0=stf[:], scalar1=tabs[:, T_PV:T_PV + 1], scalar2=0.0, op0=ALU.mult, op1=ALU.add),
                  reads=["tabs"] + skeys, writes=skeys)
    if full:
        for h in range(8):
            P.add("act", lambda e, h=h: e.activation(out=stb[:, h, :], in_=stf[:, h, :], func=AF.Copy), reads=["D_stf%d" % h], writes=["D_stb%d" % h])
    sqg = P.sb("D_sqg", [128, 8, N], BF16)
    sq = sqg
    gated = sqg
    xn = P.sb("D_xn", [128, 8, N], BF16)
    Vt = P.sb("D_Vt", [128, 4, 1024], BF16)
    NB = 2

    def mk(nm, dt=F32, n=NB):
        return [P.sb("D_%s%d" % (nm, i), [128, N], dt) for i in range(n)]
    t_f, t_kk, t_b, t_d1, t_d2, t_x = mk("f"), mk("kk"), mk("b"), mk("d1"), mk("d2"), mk("x")
    t_Qt, t_Kt, t_ks = mk("Qt", BF16), mk("Kt", BF16), mk("ks", BF16)
    sd, rstd = t_x[0], t_d1[0]
    p_ksT = mk("ksT", BF16, 8)
    decs = P.sb("D_decs", [128, 8, 8], F32)
    if full:
        p_qi, p_At, p_sg = mk("qi", BF16, 8), mk("At", BF16, 8), mk("sg", BF16, 8)
        p_o = mk("o", F32, 8)
    pj = [P.ps("D_pj%d" % i, [128, 512]) for i in range(2)]
    psS = P.ps("D_psS", [128, 512])
    psT = P.ps("D_psT", [128, 512], BF16)
    pob = [P.ps("D_po%d" % i, [128, 512]) for i in range(2)]
    pkvb = [P.ps("D_pkv%d" % i, [128, 512]) for i in range(2)]
    hv = d["h2T"].rearrange("(c p) t -> p c t", p=128)
    ov = d["h3T"].rearrange("(c p) t -> p c t", p=128)
    pc = 0
    hc = 0
    kvc = 0
    poc = 0
    for j in range(4096 // N):
        P.dma(ht[:], hv[:, :, j * N:(j + 1) * N], writes=["D_ht%d" % c for c in range(8)])
        hkeys = ["D_ht%d" % c for c in range(8)]
        P.add("act", lambda e: e.activation(out=sq[:], in_=ht[:], func=AF.Square), reads=hkeys, writes=["D_sq"])
        gen = pj[pc % 2]; gk = "D_pj%d" % (pc % 2); pc += 1
        for c in range(8):
            P.add("pe", lambda e, c=c, gen=gen: e.matmul(gen[:, :], lhsT=onesm[:], rhs=sq[:, c, :], start=(c == 0), stop=(c == 7)), reads=["D_sq", "ones_d1024"], writes=[gk])
        P.add("act", lambda e, gen=gen: e.activation(out=sd[:], in_=gen[:], func=AF.Sqrt, bias=EPS, scale=1.0), reads=[gk], writes=["D_x0"])
        P.add("dve", lambda e: e.reciprocal(out=rstd[:], in_=sd[:]), reads=["D_x0"], writes=["D_d10"])
        for c in range(8):
            P.add("dve", lambda e, c=c: stt(e, xn[:, c, :], ht[:, c, :], tabs[:, T_HGRN_NORM + c:T_HGRN_NORM + c + 1], rstd[:]),
                  reads=["D_ht%d" % c, "D_d10", "tabs"], writes=["D_xn%d" % c])
        nkeys = ["D_xn%d" % c for c in range(8)]
        for tb in range(4):
            for hf in range(2):
                gen = pj[pc % 2]; gk = "D_pj%d" % (pc % 2); pc += 1
                for c in range(8):
                    P.add("pe", lambda e, c=c, tb=tb, hf=hf, gen=gen: e.matmul(gen[:, :], lhsT=xn[:, c, tb * 128:(tb + 1) * 128], rhs=w[:, c, 2048 + hf * 512:2048 + (hf + 1) * 512],
                                                                       start=(c == 0), stop=(c == 7)), reads=["D_w"] + nkeys, writes=[gk])
                if (tb + hf) % 2 == 0:
                    P.add("act", lambda e, tb=tb, hf=hf, gen=gen: e.activation(out=Vt[:, tb, hf * 512:(hf + 1) * 512], in_=gen[:], func=AF.Copy), reads=[gk], writes=["D_Vt"])
                else:
                    P.add("dve", lambda e, tb=tb, hf=hf, gen=gen: e.tensor_copy(out=Vt[:, tb, hf * 512:(hf + 1) * 512], in_=gen[:]), reads=[gk], writes=["D_Vt"])
        def head_prologue(h, bi):
            nonlocal pc
            K = lambda nm: "D_%s%d" % (nm, bi)
            KH = lambda nm: "D_%s%d" % (nm, h)
            f, kk, b, d1, d2, xx = t_f[bi], t_kk[bi], t_b[bi], t_d1[bi], t_d2[bi], t_x[bi]
            Qt, Kt, ks = t_Qt[bi], t_Kt[bi], t_ks[bi]
            ksT = p_ksT[h]

            def proj(col0):
                nonlocal pc
                pp = pj[pc % 2]; pk = "D_pj%d" % (pc % 2); pc += 1
                for c in range(8):
                    P.add("pe", lambda e, pp=pp, c=c, col0=col0: e.matmul(pp[:, :], lhsT=w[:, c, col0:col0 + 128], rhs=xn[:, c, :], start=(c == 0), stop=(c == 7)),
                          reads=["D_w"] + nkeys, writes=[pk])
                return pp, pk
            pf, pfk = proj(1024 + h * 128)
            P.add("act", lambda e: e.activation(out=f[:], in_=pf[:], func=AF.Sigmoid), reads=[pfk], writes=[K("f")])
            P.add("dve", lambda e: e.tensor_scalar(out=kk[:], in0=f[:], scalar1=noml[:, h:h + 1], scalar2=oml[:, h:h + 1], op0=ALU.mult, op1=ALU.add),
                  reads=[K("f"), "D_oml", "D_noml"], writes=[K("kk")])
            P.add("act", lambda e: e.activation(out=f[:], in_=f[:], func=AF.Ln, bias=lb[:, h:h + 1], scale=oml[:, h:h + 1]),
                  reads=[K("f"), K("kk"), "D_oml", "D_lb"], writes=[K("f")])
            P.add("dve", lambda e: e.tensor_tensor_scan(out=b[:], data0=rm[:], data1=f[:], initial=0.0, op0=ALU.mult, op1=ALU.add),
                  reads=[K("f"), "D_rm"], writes=[K("b")])
            b3 = b[:].rearrange("p (a b) -> p a b", b=64)
            P.add("pool", lambda e: e.tensor_tensor(out=d2[:].rearrange("p (a b) -> p a b", b=64), in0=b3[:, :, 63:64].to_broadcast([128, 8, 64]), in1=b3, op=ALU.subtract),
                  reads=[K("b")], writes=[K("d2")])
            if full:
                P.add("dve", lambda e: e.tensor_tensor(out=d1[:].rearrange("p (a b) -> p a b", b=64), in0=b3, in1=b3[:, :, 32:33].to_broadcast([128, 8, 64]), op=ALU.subtract),
                      reads=[K("b")], writes=[K("d1")])
            P.add("act", lambda e: e.activation(out=decs[:, h, :].unsqueeze(2), in_=b3[:, :, 63:64], func=AF.Exp), reads=[K("b")], writes=[KH("dec")])
            P.add("act", lambda e: e.activation(out=d2[:], in_=d2[:], func=AF.Exp), reads=[K("d2")], writes=[K("d2")])
            if full:
                qi, At, sgt = p_qi[h], p_At[h], p_sg[h]
                P.add("act", lambda e: e.activation(out=xx[:], in_=d1[:], func=AF.Exp, scale=-1.0), reads=[K("d1")], writes=[K("x")])
                P.add("act", lambda e: e.activation(out=d1[:], in_=d1[:], func=AF.Exp), reads=[K("d1"), K("x")], writes=[K("d1")])
                P.add("act", lambda e: e.activation(out=b[:], in_=b[:], func=AF.Exp), reads=[K("b"), K("d2"), K("d1"), KH("dec")], writes=[K("b")])
                pq, pqk = proj(h * 128)
                P.add("act", lambda e: e.activation(out=f[:], in_=pq[:], func=AF.Silu), reads=[pqk, K("f"), K("b")], writes=[K("f")])
                pg, pgk = proj(3072 + h * 128)
                P.add("act", lambda e: e.activation(out=sgt[:], in_=pg[:], func=AF.Sigmoid), reads=[pgk], writes=[KH("sg")])

            def stageB():
                P.add("pool", lambda e: e.tensor_tensor(out=ks[:], in0=kk[:], in1=d2[:], op=ALU.mult), reads=[K("kk"), K("d2")], writes=[K("ks")])
                if full:
                    P.add("pool", lambda e: e.tensor_tensor(out=Kt[:], in0=kk[:], in1=xx[:], op=ALU.mult), reads=[K("kk"), K("x")], writes=[K("Kt")])
                    P.add("pool", lambda e: e.tensor_tensor(out=Qt[:], in0=f[:], in1=d1[:], op=ALU.mult), reads=[K("f"), K("d1")], writes=[K("Qt")])
                    P.add("pool", lambda e: e.tensor_tensor(out=qi[:], in0=f[:], in1=b[:], op=ALU.mult), reads=[K("f"), K("b")], writes=[KH("qi")])
                for pr in range(4):
                    P.add("pe", lambda e, pr=pr: e.transpose(psT[:, pr * 128:(pr + 1) * 128], ks[:, pr * 128:(pr + 1) * 128], ident[:]),
                          reads=[K("ks"), "ident"], writes=["D_psT"])
                P.add("act", lambda e: e.activation(out=ksT[:], in_=psT[:], func=AF.Copy), reads=["D_psT"], writes=[KH("ksT")])
                if full:
                    for pr in range(4):
                        P.add("pe", lambda e, pr=pr: e.matmul(psS[:, pr * 128:(pr + 1) * 128], lhsT=Kt[:, pr * 128:(pr + 1) * 128], rhs=Qt[:, pr * 128:(pr + 1) * 128], start=True, stop=True),
                              reads=[K("Kt"), K("Qt")], writes=["D_psS"])
                    P.add("dve", lambda e: e.tensor_tensor(out=At[:].rearrange("p (a b) -> p a b", b=128), in0=psS[:].rearrange("p (a b) -> p a b", b=128),
                                                           in1=m2[:].unsqueeze(1).to_broadcast([128, 4, 128]), op=ALU.mult), reads=["D_psS", "D_m2"], writes=[KH("At")])
            return stageB

        pend = None
        for h in range(8):
            bi = hc % NB
            hc += 1
            sb_ = head_prologue(h, bi)
            if pend is not None:
                pend()
            pend = sb_
        pend()
        for pr in range(4):
            if full:
                for h in range(8):
                    po_ = pob[h % 2][:, 0:128]
                    pok = "D_pob%d" % (h % 2)
                    P.add("pe", lambda e, po_=po_, pr=pr, h=h: e.matmul(po_, lhsT=Vt[:, pr, h * 128:(h + 1) * 128], rhs=p_At[h][:, pr * 128:(pr + 1) * 128], start=True, stop=True),
                          reads=["D_Vt", "D_At%d" % h], writes=[pok])
                    if h % 2 == 0:
                        P.add("act", lambda e, po_=po_, h=h, pr=pr: e.activation(out=p_o[h][:, pr * 128:(pr + 1) * 128], in_=po_, func=AF.Copy), reads=[pok], writes=["D_o%d" % h])
                    else:
                        P.add("dve", lambda e, po_=po_, h=h, pr=pr: e.tensor_copy(out=p_o[h][:, pr * 128:(pr + 1) * 128], in_=po_), reads=[pok], writes=["D_o%d" % h])
            for cc in range(2):
                c = pr * 2 + cc
                for h in range(8):
                    kvb = pkvb[h % 2]
                    kvk = "D_pkvb%d" % (h % 2)
                    kv = kvb[:, 0:128]
                    if full:
                        oi = kvb[:, 128:192]
                        P.add("pe", lambda e, oi=oi, c=c, h=h: e.matmul(oi, lhsT=stb[:, h, :], rhs=p_qi[h][:, c * 64:(c + 1) * 64], start=True, stop=True),
                              reads=["D_stb%d" % h, "D_qi%d" % h], writes=[kvk])
                    P.add("pe", lambda e, kv=kv, cc=cc, pr=pr, h=h: e.matmul(kv, lhsT=p_ksT[h][cc * 64:(cc + 1) * 64, pr * 128:(pr + 1) * 128],
                                                                            rhs=Vt[cc * 64:(cc + 1) * 64, pr, h * 128:(h + 1) * 128], start=True, stop=True),
                          reads=["D_ksT%d" % h, "D_Vt"], writes=[kvk])
                    if full:
                        P.add("dve", lambda e, oi=oi, h=h, c=c: e.tensor_tensor(out=p_o[h][:, c * 64:(c + 1) * 64], in0=oi, in1=p_o[h][:, c * 64:(c + 1) * 64], op=ALU.add),
                              reads=[kvk, "D_o%d" % h], writes=["D_o%d" % h])
                    P.add("dve", lambda e, kv=kv, h=h, c=c: stt(e, stf[:, h, :], stf[:, h, :], decs[:, h, c:c + 1], kv, op0=ALU.mult, op1=ALU.add),
                          reads=[kvk, "D_stf%d" % h, "D_dec%d" % h], writes=["D_stf%d" % h])
                    if full:
                        P.add("act", lambda e, h=h: e.activation(out=stb[:, h, :], in_=stf[:, h, :], func=AF.Copy), reads=["D_stf%d" % h], writes=["D_stb%d" % h])
        if not full:
            continue
        for h in range(8):
            bi = h % NB
            o_sb = p_o[h]
            sqo = t_Qt[bi]
            rr = t_x[bi]
            K = lambda nm: "D_%s%d" % (nm, bi)
            P.add("act", lambda e, sqo=sqo, o_sb=o_sb: e.activation(out=sqo[:], in_=o_sb[:], func=AF.Square), reads=["D_o%d" % h], writes=[K("Qt")])
            gen = pj[pc % 2]; gk = "D_pj%d" % (pc % 2); pc += 1
            P.add("pe", lambda e, sqo=sqo, gen=gen: e.matmul(gen[:, :], lhsT=ones128[:], rhs=sqo[:], start=True, stop=True), reads=[K("Qt"), "ones_d128"], writes=[gk])
            P.add("act", lambda e, rr=rr, gen=gen: e.activation(out=rr[:], in_=gen[:], func=AF.Sqrt, bias=EPS, scale=1.0), reads=[gk], writes=[K("x")])
            P.add("dve", lambda e, rr=rr: e.reciprocal(out=rr[:], in_=rr[:]), reads=[K("x")], writes=[K("x")])
            P.add("dve", lambda e, o_sb=o_sb, rr=rr: stt(e, o_sb[:], o_sb[:], tabs[:, T_HOG:T_HOG + 1], rr[:]), reads=["D_o%d" % h, K("x"), "tabs"], writes=["D_o%d" % h])
            P.add("pool", lambda e, o_sb=o_sb, h=h: e.tensor_tensor(out=gated[:, h, :], in0=o_sb[:], in1=p_sg[h][:], op=ALU.mult), reads=["D_o%d" % h, "D_sg%d" % h, "D_sq"], writes=["D_gated%d" % h])
        gkeys = ["D_gated%d" % h for h in range(8)]
        for oc in range(8):
            pp = pj[pc % 2]; pk = "D_pj%d" % (pc % 2); pc += 1
            for c in range(8):
                P.add("pe", lambda e, c=c, oc=oc, pp=pp: e.matmul(pp[:, :], lhsT=wo[:, c, oc * 128:(oc + 1) * 128], rhs=gated[:, c, :], start=(c == 0), stop=(c == 7)),
                      reads=["D_wo"] + gkeys, writes=[pk])
            P.add("dve", lambda e, oc=oc, pp=pp: e.tensor_tensor(out=ht[:, oc, :], in0=pp[:], in1=ht[:, oc, :], op=ALU.add), reads=[pk, "D_ht%d" % oc], writes=["D_ht%d" % oc])
        if write_h:
            P.dma(ov[:, :, j * N:(j + 1) * N], ht[:], reads=hkeys + gkeys)
    P.dma(d["s_out"].rearrange("h k v -> k h v"), stf[:], reads=skeys)


def phase_E(P, C, d, n_exp=8):
    tabs = C.tabs
    N = 512
    TG = 2048
    onesm = C.ones_scaled(1.0 / 1024, "d1024")
    identf = P.sb("E_identf", [128, 128], F32)
    P.add("dve", lambda e: e.tensor_copy(out=identf[:], in_=C.ident[:]), reads=["ident"], writes=["E_identf"])
    sel = P.sb("E_sel", [8, 8, 128], F32)
    P.add("pool", lambda e: e.memset(sel[:], 1.0), writes=["E_sel"])
    P.add("pool", lambda e: e.affine_select(out=sel[:], in_=sel[:], pattern=[[1, 8], [0, 128]], compare_op=ALU.is_equal, fill=0.0, base=0, channel_multiplier=-1),
          reads=["E_sel"], writes=["E_sel"])
    wr = P.sb("E_wr", [128, 8, 8], F32)
    P.dma(wr[:], d["moe_w_router"].rearrange("(c p) e -> p c e", p=128), writes=["E_wr"])
    acc = P.sb("E_acc", [128, 8, TG], F32)
    xn = P.sb("E_xn", [128, 8, TG], BF16)
    xf = P.sb("E_xf", [128, 8, N], F32)
    sq = P.sb("E_sq", [128, 8, N], BF16)
    sd = P.sb("E_sd", [128, N], F32)
    rstd = P.sb("E_rstd", [128, N], F32)
    gT = P.sb("E_gT", [8, TG], F32)
    gbt = P.sb("E_gbt", [128, TG], BF16)
    lg = P.sb("E_lg", [128, 8], F32)
    eq1 = P.sb("E_eq1", [128, 8], F32)
    eq2 = P.sb("E_eq2", [128, 8], F32)
    l2 = P.sb("E_l2", [128, 8], F32)
    m1 = P.sb("E_m1", [128, 1], F32)
    m2 = P.sb("E_m2", [128, 1], F32)
    g1 = P.sb("E_g1", [128, 1], F32)
    g2 = P.sb("E_g2", [128, 1], F32)
    gts = P.sb("E_gts", [128, 8], F32)
    wgb = [P.sb("E_wg%d" % i, [128, 8, 512], BF16) for i in range(2)]
    wub = [P.sb("E_wu%d" % i, [128, 8, 512], BF16) for i in range(2)]
    wdb = [P.sb("E_wd%d" % i, [128, 4, 1024], BF16) for i in range(2)]
    ab = [P.sb("E_a%d" % i, [128, 4, N], BF16) for i in range(2)]
    sgb = [P.sb("E_sg%d" % i, [128, N], BF16) for i in range(2)]
    t1b = [P.sb("E_t1%d" % i, [128, N], BF16) for i in range(2)]
    pgu = [P.ps("E_pgu%d" % i, [128, 512]) for i in range(4)]
    pdn = [P.ps("E_pdn%d" % i, [128, 512]) for i in range(2)]
    gen = P.ps("E_gen", [128, 512])
    pss = P.ps("E_pss", [128, 512])
    hv = d["h3T"].rearrange("(c p) t -> p c t", p=128)
    ov = d["outT"].rearrange("(c p) t -> p c t", p=128)
    wc = 0
    uc = 0
    dc = 0
    for tg in range(4096 // TG):
        for t in range(TG // N):
            cols = slice(t * N, (t + 1) * N)
            akeys = ["E_acc%d_%d" % (c, t) for c in range(8)]
            P.dma(acc[:, :, cols], hv[:, :, tg * TG + t * N:tg * TG + (t + 1) * N], writes=akeys)
            P.add("act", lambda e, cols=cols: e.activation(out=sq[:], in_=acc[:, :, cols], func=AF.Square), reads=akeys, writes=["E_sq"])
            for c in range(8):
                P.add("pe", lambda e, c=c: e.matmul(pss[:, :], lhsT=onesm[:], rhs=sq[:, c, :], start=(c == 0), stop=(c == 7)), reads=["E_sq", "ones_d1024"], writes=["E_pss"])
            P.add("act", lambda e: e.activation(out=sd[:], in_=pss[:], func=AF.Sqrt, bias=EPS, scale=1.0), reads=["E_pss"], writes=["E_sd"])
            P.add("dve", lambda e: e.reciprocal(out=rstd[:], in_=sd[:]), reads=["E_sd"], writes=["E_rstd"])
            for c in range(8):
                P.add("dve", lambda e, c=c, cols=cols: stt(e, xf[:, c, :], acc[:, c, cols], tabs[:, T_MOE_NORM + c:T_MOE_NORM + c + 1], rstd[:]),
                      reads=["E_acc%d_%d" % (c, t), "E_rstd", "tabs"], writes=["E_xf%d" % c])
                P.add("act", lambda e, c=c, cols=cols: e.activation(out=xn[:, c, cols], in_=xf[:, c, :], func=AF.Copy), reads=["E_xf%d" % c], writes=["E_xn%d_%d" % (c, t)])
            fkeys = ["E_xf%d" % c for c in range(8)]
            for tb in range(4):
                for c in range(8):
                    P.add("pe", lambda e, c=c, tb=tb: e.matmul(gen[:, 0:8], lhsT=xf[:, c, tb * 128:(tb + 1) * 128], rhs=wr[:, c, :], start=(c == 0), stop=(c == 7)),
                          reads=fkeys + ["E_wr"], writes=["E_gen"])
                P.add("dve", lambda e: e.tensor_copy(out=lg[:], in_=gen[:, 0:8]), reads=["E_gen"], writes=["E_lg"])
                P.add("dve", lambda e: e.reduce_max(out=m1[:], in_=lg[:], axis=AX.X), reads=["E_lg"], writes=["E_m1"])
                P.add("dve", lambda e: e.tensor_tensor(out=eq1[:], in0=lg[:], in1=m1[:, 0:1].to_broadcast([128, 8]), op=ALU.is_equal), reads=["E_lg", "E_m1"], writes=["E_eq1"])
                P.add("dve", lambda e: stt(e, l2[:], eq1[:], -1e30, lg[:], op0=ALU.mult, op1=ALU.add), reads=["E_eq1", "E_lg"], writes=["E_l2"])
                P.add("dve", lambda e: e.reduce_max(out=m2[:], in_=l2[:], axis=AX.X), reads=["E_l2"], writes=["E_m2"])
                P.add("dve", lambda e: e.tensor_tensor(out=eq2[:], in0=l2[:], in1=m2[:, 0:1].to_broadcast([128, 8]), op=ALU.is_equal), reads=["E_l2", "E_m2"], writes=["E_eq2"])
                P.add("dve", lambda e: e.tensor_tensor(out=g1[:], in0=m2[:], in1=m1[:], op=ALU.subtract), reads=["E_m1", "E_m2"], writes=["E_g1"])
                P.add("act", lambda e: e.activation(out=g1[:], in_=g1[:], func=AF.Exp), reads=["E_g1"], writes=["E_g1"])
                P.add("dve", lambda e: e.tensor_scalar(out=g1[:], in0=g1[:], scalar1=1.0, scalar2=0.0, op0=ALU.add, op1=ALU.add), reads=["E_g1"], writes=["E_g1"])
                P.add("dve", lambda e: e.reciprocal(out=g1[:], in_=g1[:]), reads=["E_g1"], writes=["E_g1"])
                P.add("dve", lambda e: e.tensor_scalar(out=g2[:], in0=g1[:], scalar1=-1.0, scalar2=1.0, op0=ALU.mult, op1=ALU.add), reads=["E_g1"], writes=["E_g2"])
                P.add("dve", lambda e: e.tensor_scalar(out=gts[:], in0=eq1[:], scalar1=g1[:, 0:1], scalar2=0.0, op0=ALU.mult, op1=ALU.add), reads=["E_eq1", "E_g1"], writes=["E_gts"])
                P.add("dve", lambda e: stt(e, gts[:], eq2[:], g2[:, 0:1], gts[:], op0=ALU.mult, op1=ALU.add), reads=["E_eq2", "E_g2", "E_gts"], writes=["E_gts"])
                P.add("pe", lambda e: e.transpose(gen[0:8, 128:256], gts[:, :], identf[:]), reads=["E_gts", "E_identf"], writes=["E_gen"])
                c0 = t * N + tb * 128
                P.add("act", lambda e, c0=c0: e.activation(out=gT[:, c0:c0 + 128], in_=gen[0:8, 128:256], func=AF.Copy), reads=["E_gen"], writes=["E_gT"])
        xkeys_t = [["E_xn%d_%d" % (c, t) for c in range(8)] for t in range(TG // N)]
        units = [(ex, fg) for ex in range(n_exp) for fg in range(7)]

        def w_chunks(u):
            ex, fg = units[u]
            wi = u % 2
            wg, wu, wd = wgb[wi], wub[wi], wdb[wi]
            lst = []
            for c in range(8):
                lst.append((wg[:, c, :], d["moe_w_gate"][ex, c * 128:(c + 1) * 128, fg * 512:(fg + 1) * 512], "E_wg%d" % wi))
            for c in range(8):
                lst.append((wu[:, c, :], d["moe_w_up"][ex, c * 128:(c + 1) * 128, fg * 512:(fg + 1) * 512], "E_wu%d" % wi))
            for c in range(4):
                for hf in range(2):
                    lst.append((wd[:, c, hf * 512:(hf + 1) * 512], d["moe_w_down"][ex, fg * 512 + c * 128:fg * 512 + (c + 1) * 128, hf * 512:(hf + 1) * 512], "E_wd%d" % wi))
            return lst

        slot_ctr = [0]

        def load_group(u, gi, do_dma=True, do_cast=True, _st={}):
            lst = w_chunks(u)[7 * gi:7 * gi + 7]
            if do_dma:
                sl = []
                for (dst, src, key) in lst:
                    si_ = slot_ctr[0] % 8
                    slot_ctr[0] += 1
                    P.dma(xf[:, si_, :], src, writes=["E_xf%d" % si_])
                    sl.append(si_)
                _st[(u, gi)] = sl
            if do_cast:
                for (dst, src, key), si_ in zip(lst, _st[(u, gi)]):
                    P.add("act", lambda e, dst=dst, si_=si_: e.activation(out=dst, in_=xf[:, si_, :], func=AF.Copy), reads=["E_xf%d" % si_], writes=[key])

        def load_w(u):
            for gi in range(4):
                load_group(u, gi)

        load_w(0)
        pend_down = [None]
        for u, (ex, fg) in enumerate(units):
            if fg == 0:
                for t in range(TG // N):
                    P.add("pe", lambda e, ex=ex, t=t: e.matmul(gen[:, :], lhsT=sel[:, ex, :], rhs=gT[:, t * N:(t + 1) * N], start=True, stop=True), reads=["E_sel", "E_gT"], writes=["E_gen"])
                    P.add("act", lambda e, t=t: e.activation(out=gbt[:, t * N:(t + 1) * N], in_=gen[:], func=AF.Copy), reads=["E_gen"], writes=["E_gbt"])
            if True:
                wi = u % 2
                wg, wu, wd = wgb[wi], wub[wi], wdb[wi]
                for t in range(TG // N):
                    cols = slice(t * N, (t + 1) * N)
                    ai = uc % 2
                    a = ab[ai]
                    if u + 1 < len(units):
                        load_group(u + 1, t, do_dma=True, do_cast=False)
                    for fc in range(4):
                        pg = pgu[(uc * 8 + fc * 2) % 4]; pgk = "E_pgu%d" % ((uc * 8 + fc * 2) % 4)
                        pu = pgu[(uc * 8 + fc * 2 + 1) % 4]; puk = "E_pgu%d" % ((uc * 8 + fc * 2 + 1) % 4)
                        for c in range(8):
                            P.add("pe", lambda e, pg=pg, c=c, fc=fc, wg=wg, cols=cols: e.matmul(pg[:, :], lhsT=wg[:, c, fc * 128:(fc + 1) * 128], rhs=xn[:, c, cols], start=(c == 0), stop=(c == 7)),
                                  reads=["E_wg%d" % wi] + xkeys_t[t], writes=[pgk])
                        for c in range(8):
                            P.add("pe", lambda e, pu=pu, c=c, fc=fc, wu=wu, cols=cols: e.matmul(pu[:, :], lhsT=wu[:, c, fc * 128:(fc + 1) * 128], rhs=xn[:, c, cols], start=(c == 0), stop=(c == 7)),
                                  reads=["E_wu%d" % wi] + xkeys_t[t], writes=[puk])
                        si = fc % 2
                        P.add("act", lambda e, pg=pg, si=si: e.activation(out=sgb[si][:], in_=pg[:], func=AF.Silu), reads=[pgk], writes=["E_sg%d" % si])
                        P.add("pool", lambda e, si=si, cols=cols: e.tensor_tensor(out=t1b[si][:], in0=sgb[si][:], in1=gbt[:, cols], op=ALU.mult), reads=["E_sg%d" % si, "E_gbt"], writes=["E_t1%d" % si])
                        P.add("dve", lambda e, pu=pu, si=si, a=a, fc=fc: e.tensor_tensor(out=a[:, fc, :], in0=pu[:], in1=t1b[si][:], op=ALU.mult), reads=[puk, "E_t1%d" % si], writes=["E_a%d_%d" % (ai, fc)])
                    uc += 1

                    def down(a=a, ai=ai, wd=wd, wi=wi, cols=cols, t=t):
                        nonlocal dc
                        for oc in range(8):
                            pd = pdn[dc % 2]; pdk = "E_pdn%d" % (dc % 2); dc += 1
                            for fc in range(4):
                                P.add("pe", lambda e, pd=pd, fc=fc, oc=oc: e.matmul(pd[:, :], lhsT=wd[:, fc, oc * 128:(oc + 1) * 128], rhs=a[:, fc, :], start=(fc == 0), stop=(fc == 3)),
                                      reads=["E_wd%d" % wi] + ["E_a%d_%d" % (ai, f_) for f_ in range(4)], writes=[pdk])
                            P.add("dve", lambda e, pd=pd, oc=oc: e.tensor_tensor(out=acc[:, oc, cols], in0=pd[:], in1=acc[:, oc, cols], op=ALU.add),
                                  reads=[pdk, "E_acc%d_%d" % (oc, t)], writes=["E_acc%d_%d" % (oc, t)])
                    if pend_down[0] is not None:
                        pend_down[0]()
                    pend_down[0] = down
                    if u + 1 < len(units):
                        load_group(u + 1, t, do_dma=False, do_cast=True)
        pend_down[0]()
        pend_down[0] = None
        P.dma(ov[:, :, tg * TG:(tg + 1) * TG], acc[:], reads=["E_acc%d_%d" % (c, t) for c in range(8) for t in range(4)])


SCR = [("qd", [768, 4096]), ("kd", [768, 6144]), ("qf1", [256, 4096]), ("qf2", [256, 4096]), ("kf", [256, 8192]),
       ("vd", [12, 128, 48, 65]), ("vf", [4, 128, 64, 65])]
W_IN = [("attn_w_in", [1024, 3072]), ("attn_w_out", [1024, 1024]), ("ffn_w_gate", [1024, 2816]), ("ffn_w_up", [1024, 2816]),
        ("ffn_w_down", [2816, 1024]), ("hgrn_w_in", [1024, 4096]), ("hgrn_w_out", [1024, 1024]), ("moe_w_router", [1024, 8]),
        ("moe_w_gate", [8, 1024, 3584]), ("moe_w_up", [8, 1024, 3584]), ("moe_w_down", [8, 3584, 1024])]


def build_fused(n_cores=8, phases="ABCDE", n_exp=8):
    nc = bass.Bass("TRN2", target_bir_lowering=False)
    d = {}
    tabs = nc.dram_tensor("tabs", [128, T_NT], F32, kind="ExternalInput").ap()
    d["xT"] = nc.dram_tensor("xT", [1024, 8192], F32, kind="ExternalInput").ap()
    for n, s in W_IN:
        d[n] = nc.dram_tensor(n, s, F32, kind="ExternalInput").ap()
    d["outT"] = nc.dram_tensor("outT", [1024, 4096], F32, kind="ExternalOutput").ap()
    for n, s in SCR:
        d[n] = nc.dram_tensor(n, s, BF16).ap()
    d["mixT"] = nc.dram_tensor("mixT", [1024, 4096], BF16).ap()
    d["h2T"] = nc.dram_tensor("h2T", [1024, 4096], F32).ap()
    d["h3T"] = nc.dram_tensor("h3T", [1024, 4096], F32).ap()
    s_a = nc.dram_tensor("s_a", [1024, 128], F32).ap()
    s_g = nc.dram_tensor("s_g", [2048, 128], F32).ap()
    s_dummy = nc.dram_tensor("s_dummy", [1024, 128], F32).ap()

    def run_phase(tag, fn):
        with nc.cleanup_on_exit():
            P = Prog(nc, tag=tag, fused=True)
            C = Ctx(P, tabs)
            fn(P, C)
            P.emit()
            nc.all_engine_barrier()

    if "A" in phases:
        run_phase("A_", lambda P, C: phase_A(P, C, d))
    if "B" in phases:
        run_phase("B_", lambda P, C: phase_B(P, C, d))
    if "C" in phases:
        run_phase("C_", lambda P, C: phase_C(P, C, d))
    if "D" in phases:
        d1 = dict(d); d1["s_out"] = s_a.rearrange("(h k) v -> h k v", h=8)
        run_phase("D1_", lambda P, C: phase_D(P, C, d1, zero_init=True, write_h=False, state_only=True))
        groups = [[2 * i, 2 * i + 1] for i in range(n_cores // 2)]
        with nc.cleanup_on_exit():
            cc_sem = nc.alloc_semaphore("cc_sem")
            with nc.Block() as block:
                @block.gpsimd
                def _(g):
                    g.collective_compute("AllGather", ALU.bypass, replica_groups=groups, ins=[s_a], outs=[s_g]).then_inc(cc_sem)
                    g.wait_ge(cc_sem, 1)
            nc.all_engine_barrier()
        d2 = dict(d); d2["s_in"] = s_g[0:1024, :].rearrange("(h k) v -> h k v", h=8); d2["s_out"] = s_dummy.rearrange("(h k) v -> h k v", h=8)
        run_phase("D2_", lambda P, C: phase_D(P, C, d2, scale_pv=True))
    if "E" in phases:
        run_phase("E_", lambda P, C: phase_E(P, C, d, n_exp=n_exp))
    return nc


def make_tabs(inp, pv):
    t = np.zeros((128, T_NT), np.float32)
    p = np.arange(128)
    def pc(v):
        return np.asarray(v, np.float32).reshape(8, 128).T
    t[:, T_ATTN_NORM:T_ATTN_NORM + 8] = pc(inp["attn_norm"][0])
    t[:, T_FFN_NORM:T_FFN_NORM + 8] = pc(inp["ffn_norm"][0])
    t[:, T_HGRN_NORM:T_HGRN_NORM + 8] = pc(inp["hgrn_norm"][0])
    t[:, T_MOE_NORM:T_MOE_NORM + 8] = pc(inp["moe_norm"][0])
    t[:, T_DQG] = inp["dil_q_gain"][0][p % 64]
    t[:, T_DKG] = inp["dil_k_gain"][0][p % 64]
    t[:, T_FQG] = inp["diff_q_gain"][0][p % 32]
    t[:, T_FKG] = inp["diff_k_gain"][0][p % 32]
    t[:, T_DOG] = inp["diff_out_gain"][0][p % 64]
    t[:, T_HOG] = inp["hgrn_out_gain"][0]
    t[:, T_PV] = pv
    t[:, T_M1] = (p % 64 < 32)
    t[:, T_M2] = (p % 64 >= 32)
    t[:, T_LB0:T_LB0 + 8] = pc(inp["hgrn_lb_logits"][0])
    t[:, T_LB1:T_LB1 + 8] = pc(inp["hgrn_lb_logits"][1])
    for i, k in enumerate(["diff_lambda_q1", "diff_lambda_k1", "diff_lambda_q2", "diff_lambda_k2"]):
        t[:32, T_LQ1 + i] = inp[k][0]
    return t

def core_xT(x, core):
    b, hf = core // 2, core % 2
    xb = x[b]
    out = np.zeros((1024, 8192), np.float32)
    if hf == 1:
        out[:, :4096] = xb[:4096].T
        out[:, 4096:] = xb[4096:].T
    else:
        out[:, 4096:] = xb[:4096].T
    return out


_NC = {}


def kernel(**inp):
    inp = {k: np.asarray(v) for k, v in inp.items()}
    NCORE = 8
    cores = list(range(NCORE))
    if "nc" not in _NC:
        _NC["nc"] = build_fused(NCORE)
    maps = []
    for c in cores:
        m = {"tabs": make_tabs(inp, c % 2), "xT": core_xT(inp["x"], c)}
        for n, s in W_IN:
            m[n] = np.ascontiguousarray(inp[n][0])
        maps.append(m)
    res = run_bass_kernel_spmd(_NC["nc"], maps, core_ids=cores).results
    out = np.empty((4, 8192, 1024), np.float32)
    for c in cores:
        out[c // 2, (c % 2) * 4096:(c % 2 + 1) * 4096, :] = np.asarray(res[c]["outT"]).T
    return out
```

```python
import numpy as np
from contextlib import ExitStack
import concourse.bass as bass
import concourse.mybir as mybir
from concourse.bass_utils import run_bass_kernel_spmd

F32 = mybir.dt.float32
BF16 = mybir.dt.bfloat16
I32 = mybir.dt.int32
AF = mybir.ActivationFunctionType
ALU = mybir.AluOpType
AX = mybir.AxisListType

COMPUTE = ("pe", "act", "dve", "pool")
STRICT = True


class _Op:
    __slots__ = ("eng", "fn", "deps", "dma", "semkey", "val", "need_inc")


class Prog:
    def __init__(self, nc, tag="", fused=False):
        self.nc = nc
        self.tag = tag
        self.fused = fused
        self.ops = []
        self.last_w = {}
        self.readers = {}
        self.dma_cnt = {}
        self.es = ExitStack()

    def sb(self, name, shape, dt):
        return self.es.enter_context(self.nc.sbuf_tensor(self.tag + name, list(shape), dt))

    def ps(self, name, shape, dt=F32):
        return self.es.enter_context(self.nc.psum_tensor(self.tag + name, list(shape), dt))

    def add(self, eng, fn, reads=(), writes=(), dma=False, semkey=None):
        op = _Op()
        op.eng = eng
        op.fn = fn
        op.dma = dma
        op.need_inc = False
        op.val = None
        idx = len(self.ops)
        deps = {}
        for k in reads:
            w = self.last_w.get(k)
            if w is not None:
                deps[w] = True
        for k in writes:
            w = self.last_w.get(k)
            if w is not None:
                deps.setdefault(w, False)
            for r in self.readers.get(k, ()):
                deps.setdefault(r, False)
        op.deps = deps
        if dma:
            op.semkey = semkey if semkey is not None else (
                "dma:" + str(writes[0] if writes else reads[0]))
            self.dma_cnt[op.semkey] = self.dma_cnt.get(op.semkey, 0) + 1
            op.val = 16 * self.dma_cnt[op.semkey]
        self.ops.append(op)
        for k in writes:
            self.last_w[k] = idx
            self.readers[k] = []
        for k in reads:
            if k not in writes:
                self.readers.setdefault(k, []).append(idx)
        return idx

    def dma(self, out, in_, reads=(), writes=(), q="sp", semkey=None, **kw):
        def fn(e):
            return e.dma_start(out=out, in_=in_, **kw)
        return self.add(q, fn, reads, writes, dma=True, semkey=semkey)

    def emit(self):
        nc = self.nc
        ops = self.ops
        for i, op in enumerate(ops):
            for d, raw in op.deps.items():
                p = ops[d]
                if p.dma:
                    continue
                if p.eng == op.eng and not op.dma and (op.eng == "pe" or (not raw and not STRICT)):
                    continue
                p.need_inc = True
        cnt = {e: 0 for e in COMPUTE}
        for op in ops:
            if not op.dma and op.need_inc:
                cnt[op.eng] += 1
                op.val = cnt[op.eng]
        sems = {}
        def mksem(nm):
            if self.fused:
                return nc.alloc_semaphore(self.tag + nm)
            return self.es.enter_context(nc.semaphore(self.tag + nm))
        for e in COMPUTE:
            sems[e] = mksem("s_" + e)
        dkeys = sorted(self.dma_cnt.keys())
        for i, k in enumerate(dkeys):
            sems[k] = mksem("d%d" % i)
        engines = {}
        for i, op in enumerate(ops):
            engines.setdefault(op.eng, []).append(i)
        waited = {e: {} for e in engines}
        plan = {}
        for i, op in enumerate(ops):
            need = {}
            for d, raw in op.deps.items():
                p = ops[d]
                if p.dma:
                    key = p.semkey
                else:
                    if p.eng == op.eng and not op.dma and (op.eng == "pe" or (not raw and not STRICT)):
                        continue
                    key = p.eng
                v = p.val
                if v is None:
                    continue
                if need.get(key, 0) < v:
                    need[key] = v
            w = waited[op.eng]
            lst = []
            for key, v in need.items():
                if w.get(key, 0) < v:
                    w[key] = v
                    lst.append((key, v))
            plan[i] = lst
        final = [(k, 16 * n) for k, n in self.dma_cnt.items()]
        self.n_instr = len(ops)
        with nc.Block() as block:
            def run(engname, e):
                for i in engines.get(engname, ()):
                    op = ops[i]
                    for key, v in plan[i]:
                        e.wait_ge(sems[key], v)
                    ins = op.fn(e)
                    if op.dma:
                        ins.then_inc(sems[op.semkey], 16)
                    elif op.need_inc:
                        ins.then_inc(sems[op.eng], 1)
                if engname == "sp":
                    for k, v in final:
                        e.wait_ge(sems[k], v)

            @block.sync
            def _(e):
                run("sp", e)

            @block.tensor
            def _(e):
                run("pe", e)

            @block.scalar
            def _(e):
                run("act", e)

            @block.vector
            def _(e):
                run("dve", e)

            @block.gpsimd
            def _(e):
                run("pool", e)
        self.es.close()

import math

EPS = 1e-6
SLOPES = [2.0 ** (-8.0 * (h + 1) / 16) for h in range(16)]
DILS = [1, 4, 16]


def stt(e, out, in0, scalar, in1, op0=ALU.mult, op1=ALU.mult):
    return e.scalar_tensor_tensor(out=out, in0=in0, scalar=scalar, in1=in1, op0=op0, op1=op1)


class Ctx:
    def __init__(self, P, tabs_dram):
        self.P = P
        nc = P.nc
        self.tabs = P.sb("tabs_sb", [128, tabs_dram.shape[1]], F32)
        P.dma(self.tabs[:], tabs_dram, writes=["tabs"])
        self.ident = P.sb("ident", [128, 128], BF16)
        P.add("pool", lambda e: e.memset(self.ident[:], 0.0), writes=["ident"])
        P.add("pool", lambda e: e.affine_select(out=self.ident[:], in_=self.ident[:], pattern=[[-1, 128]],
                                                compare_op=ALU.not_equal, fill=1.0, base=0, channel_multiplier=1),
              reads=["ident"], writes=["ident"])
        self.ones = {}

    def ones_scaled(self, val, name):
        if name not in self.ones:
            t = self.P.sb("ones_" + name, [128, 128], BF16)
            self.P.add("pool", lambda e: e.memset(t[:], val), writes=["ones_" + name])
            self.ones[name] = t
        return self.ones[name]

    def blockdiag(self, blk, val, name):
        if name not in self.ones:
            t = self.P.sb("bd_" + name, [128, 128], BF16)
            key = "bd_" + name
            self.P.add("pool", lambda e: e.memset(t[:], val), writes=[key])
            for b in range(128 // blk):
                sl = t[:, b * blk:(b + 1) * blk]
                self.P.add("pool", lambda e, sl=sl, b=b: e.affine_select(out=sl, in_=sl, pattern=[[0, blk]],
                                                                       compare_op=ALU.is_ge, fill=0.0, base=-b * blk,
                                                                       channel_multiplier=1), reads=[key], writes=[key])
                self.P.add("pool", lambda e, sl=sl, b=b: e.affine_select(out=sl, in_=sl, pattern=[[0, blk]],
                                                                       compare_op=ALU.is_ge, fill=0.0,
                                                                       base=(b + 1) * blk - 1, channel_multiplier=-1),
                           reads=[key], writes=[key])
            self.ones[name] = t
        return self.ones[name]


def rms_tile(P, C, tag, xt, gcol0, xn_out, N, ps_key, ps, sq, sd, rstd, xn_key, x_key, xf_out=None):
    onesm = C.ones_scaled(1.0 / 1024, "d1024")
    _xa = xt(None); _xi = [xt(c) for c in range(8)]; _xo = [xn_out(c) for c in range(8)]
    _xf = [xf_out(c) for c in range(8)] if xf_out is not None else None
    xt = lambda c: _xa if c is None else _xi[c]
    xn_out = lambda c: _xo[c]
    if _xf is not None:
        xf_out = lambda c: _xf[c]
    P.add("act", lambda e: e.activation(out=sq[:, :, :N], in_=xt(None), func=AF.Square), reads=[x_key], writes=[tag + "sq"])
    for c in range(8):
        P.add("pe", lambda e, c=c: e.matmul(ps[:, :N], lhsT=onesm[:], rhs=sq[:, c, :N], start=(c == 0), stop=(c == 7)),
              reads=[tag + "sq", "ones_d1024"], writes=[ps_key])
    P.add("act", lambda e: e.activation(out=sd[:, :N], in_=ps[:, :N], func=AF.Sqrt, bias=EPS, scale=1.0),
          reads=[ps_key], writes=[tag + "sd"])
    P.add("dve", lambda e: e.reciprocal(out=rstd[:, :N], in_=sd[:, :N]), reads=[tag + "sd"], writes=[tag + "rstd"])
    for c in range(8):
        eng = "dve"
        if xf_out is not None:
            P.add(eng, lambda e, c=c: stt(e, xf_out(c), xt(c), C.tabs[:, gcol0 + c:gcol0 + c + 1], rstd[:, :N]),
                  reads=[x_key, tag + "rstd", "tabs"], writes=[xn_key + "f%d" % c])
            P.add("act", lambda e, c=c: e.activation(out=xn_out(c), in_=xf_out(c), func=AF.Copy),
                  reads=[xn_key + "f%d" % c], writes=[xn_key + "%d" % c])
        else:
            P.add(eng, lambda e, c=c: stt(e, xn_out(c), xt(c), C.tabs[:, gcol0 + c:gcol0 + c + 1], rstd[:, :N]),
                  reads=[x_key, tag + "rstd", "tabs"], writes=[xn_key + "%d" % c])


def load_w_hw(P, name, dram2d, K, N, slots, slot_keys, blk=512, keyfn=None, extra_slot_reads=()):
    kc = K // 128
    w = P.sb(name, [128, kc, N], BF16)
    if keyfn is None:
        keyfn = lambda c, col0: "%s_cb%d" % (name, col0 // 512)
    st = P.__dict__.setdefault("_slot_ctr", [0])
    engs = ("act", "dve", "pool")
    for col0 in range(0, N, blk):
        wd_ = min(blk, N - col0)
        for c in range(kc):
            si = st[0] % len(slots)
            st[0] += 1
            P.dma(slots[si][:, 0:wd_], dram2d[c * 128:(c + 1) * 128, col0:col0 + wd_], writes=[slot_keys[si]])
            eng = engs[st[0] % 3]
            dst = w[:, c, col0:col0 + wd_]
            src = slots[si][:, 0:wd_]
            if eng == "act":
                P.add("act", lambda e, dst=dst, src=src: e.activation(out=dst, in_=src, func=AF.Copy), reads=[slot_keys[si]], writes=[keyfn(c, col0)])
            else:
                P.add(eng, lambda e, dst=dst, src=src: e.tensor_copy(out=dst, in_=src), reads=[slot_keys[si]], writes=[keyfn(c, col0)])
    return w


T_ATTN_NORM = 0
T_FFN_NORM = 8
T_HGRN_NORM = 16
T_MOE_NORM = 24
T_DQG = 32
T_DKG = 33
T_FQG = 34
T_FKG = 35
T_DOG = 36
T_HOG = 37
T_PV = 38
T_M1 = 39
T_M2 = 40
T_LB0 = 41
T_LB1 = 49
T_LQ1 = 57
T_NT = 61


def phase_A(P, C, d):
    nc = P.nc
    xT = d["xT"]
    xTv = xT.rearrange("(c p) t -> p c t", p=128)
    xn1 = P.sb("A_xn", [128, 8, 2048], BF16)
    xt = P.sb("A_xt", [128, 8, 512], F32)
    xslot_keys = ["A_xt_s%d" % i for i in range(8)]
    w = load_w_hw(P, "A_w", d["attn_w_in"], 1024, 3072, [xt[:, i, :] for i in range(8)], xslot_keys,
                  keyfn=lambda c, col0: "A_w")
    sq = P.sb("A_sq", [128, 8, 512], BF16)
    sd = P.sb("A_sd", [128, 512], F32)
    rstd = P.sb("A_rstd", [128, 512], F32)
    sq2 = [P.sb("A_sq2_%d" % i, [128, 512], BF16) for i in range(2)]
    sd2 = [P.sb("A_sd2_%d" % i, [128, 512], F32) for i in range(2)]
    r2 = [P.sb("A_r2_%d" % i, [128, 512], F32) for i in range(2)]
    osts = [P.sb("A_ost%d" % i, [128, 14, 512], BF16) for i in range(2)]
    ost2 = P.sb("A_ost2", [128, 4, 2048], BF16)
    vst = P.sb("A_vst", [128, 12, 16, 65], BF16)
    vfst = P.sb("A_vfst", [128, 4, 16, 65], BF16)
    fac = P.sb("A_fac", [128, 4], F32)
    facv = P.sb("A_facv", [128, 4], F32)
    fac256 = P.sb("A_fac256", [128, 4, 64], F32)
    iop = P.sb("A_iop", [128, 1], F32)
    gq1 = P.sb("A_gq", [128, 2], F32)
    ps_ss = P.ps("A_ps_ss", [128, 512])
    pj = [P.ps("A_pj%d" % i, [128, 512]) for i in range(4)]
    ms = [P.ps("A_ms%d" % i, [128, 512]) for i in range(2)]
    bd64 = C.blockdiag(64, 1.0 / 64, "bd64")
    bd32 = C.blockdiag(32, 1.0 / 32, "bd32")
    tabs = C.tabs
    P.add("pool", lambda e: e.iota(iop[:], pattern=[[0, 1]], base=0, channel_multiplier=1,
                                   allow_small_or_imprecise_dtypes=True), writes=["A_iop"])
    for hh in range(4):
        P.add("act", lambda e, hh=hh: e.activation(out=fac[:, hh:hh + 1], in_=iop[:], func=AF.Exp, scale=SLOPES[12 + hh]),
              reads=["A_iop"], writes=["A_fac"])
    P.add("dve", lambda e: e.tensor_scalar(out=facv[:], in0=fac[:], scalar1=tabs[:, T_PV:T_PV + 1], scalar2=0.0,
                                           op0=ALU.mult, op1=ALU.add), reads=["A_fac", "tabs"], writes=["A_facv"])
    P.add("dve", lambda e: e.tensor_copy(out=fac256[:], in_=fac[:].unsqueeze(2).to_broadcast([128, 4, 64])),
          reads=["A_fac"], writes=["A_fac256"])
    P.add("dve", lambda e: e.tensor_tensor(out=gq1[:, 0:1], in0=tabs[:, T_FQG:T_FQG + 1], in1=tabs[:, T_M1:T_M1 + 1], op=ALU.mult),
          reads=["tabs"], writes=["A_gq"])
    P.add("dve", lambda e: e.tensor_tensor(out=gq1[:, 1:2], in0=tabs[:, T_FQG:T_FQG + 1], in1=tabs[:, T_M2:T_M2 + 1], op=ALU.mult),
          reads=["tabs"], writes=["A_gq"])

    cnt = {"pj": 0, "ms": 0, "tmp": 0}
    qtmp = [P.sb("A_qtmp%d" % i, [128, 512], F32) for i in range(2)]

    def qk_chunk(j, st, fc, bd, bdname, outs, xnb, col0, rr=None):
        jl_ = j % 4
        pi = cnt["pj"] % 4
        cnt["pj"] += 1
        mi = cnt["ms"] % 2
        cnt["ms"] += 1
        pk = "A_pj%d" % pi
        for c in range(8):
            P.add("pe", lambda e, c=c: e.matmul(pj[pi][:], lhsT=w[:, c, fc * 128:(fc + 1) * 128],
                                                rhs=xnb[:, c, col0:col0 + 512], start=(c == 0), stop=(c == 7)),
                  reads=["A_w", "A_xn_%d_%d" % (jl_, c)], writes=[pk])
        P.add("act", lambda e: e.activation(out=sq2[mi][:], in_=pj[pi][:], func=AF.Square), reads=[pk], writes=["A_sq2_%d" % mi])
        P.add("pe", lambda e: e.matmul(ms[mi][:], lhsT=bd[:], rhs=sq2[mi][:], start=True, stop=True),
              reads=["A_sq2_%d" % mi, "bd_" + bdname], writes=["A_ms%d" % mi])
        P.add("act", lambda e: e.activation(out=sd2[mi][:], in_=ms[mi][:], func=AF.Sqrt, bias=EPS, scale=1.0),
              reads=["A_ms%d" % mi], writes=["A_sd2_%d" % mi])
        P.add("dve", lambda e: e.reciprocal(out=r2[mi][:], in_=sd2[mi][:]), reads=["A_sd2_%d" % mi], writes=["A_r2_%d" % mi])
        def v3(ap):
            return ap if rr is None else ap.rearrange("p (u r) -> p u r", r=rr)
        for (g_ap, dst, dkey) in outs:
            ti = cnt["tmp"] % 2
            cnt["tmp"] += 1
            tmp = qtmp[ti]
            P.add("act", lambda e, g_ap=g_ap, tmp=tmp: e.activation(out=tmp[:], in_=pj[pi][:], func=AF.Copy, scale=g_ap),
                  reads=[pk, "tabs", "A_gq"], writes=["A_qtmp%d" % ti])
            P.add("pool", lambda e, dst=dst, tmp=tmp: e.tensor_tensor(out=dst, in0=v3(tmp[:]), in1=v3(r2[mi][:]), op=ALU.mult),
                  reads=["A_qtmp%d" % ti, "A_r2_%d" % mi], writes=[dkey])

    def perm_dst(buf_ap_full, g, jl):
        d_ = DILS[g]
        if g == 0:
            return buf_ap_full
        if g == 1:
            return buf_ap_full.rearrange("p (r u) -> p u r", r=4)
        return buf_ap_full.rearrange("p (r u) -> p u r", r=16)[:, jl * 32:(jl + 1) * 32, :]

    for st in range(4):
        xnb = xn1
        own = st >= 2
        need_dil = st >= 1
        for jl in range(4):
            j = st * 4 + jl
            ost = osts[j % 2]
            okey = "A_ost%d" % (j % 2)
            P.dma(xt[:], xTv[:, :, j * 512:(j + 1) * 512], writes=["A_xt"] + xslot_keys)
            col0 = jl * 512
            rms_tile(P, C, "A_", lambda c: (xt[:] if c is None else xt[:, c, :]), T_ATTN_NORM,
                     lambda c: xnb[:, c, col0:col0 + 512], 512, "A_ps_ss", ps_ss, sq, sd, rstd,
                     "A_xn_%d_" % jl, "A_xt")
            if own:
                for g in range(3):
                    for cc in range(2):
                        fc = g * 2 + cc
                        if g < 2:
                            dst = perm_dst(ost[:, g * 2 + cc, :], g, jl)
                            key = okey
                        else:
                            dst = perm_dst(ost2[:, cc, :], g, jl)
                            key = "A_ost2"
                        src_pj = None
                        qk_chunk(j, st, fc, bd64, "bd64", [(tabs[:, T_DQG:T_DQG + 1], dst, key)], xnb, col0, rr=(None, 4, 16)[g])
                for cc in range(2):
                    fc = 18 + cc
                    qk_chunk(j, st, fc, bd32, "bd32", [(gq1[:, 0:1], ost[:, 8 + cc, :], okey),
                                                       (gq1[:, 1:2], ost[:, 10 + cc, :], okey)], xnb, col0)
            if need_dil:
                for g in range(3):
                    for cc in range(2):
                        fc = 6 + g * 2 + cc
                        if g < 2:
                            dst = perm_dst(ost[:, 4 + g * 2 + cc, :], g, jl)
                            key = okey
                        else:
                            dst = perm_dst(ost2[:, 2 + cc, :], g, jl)
                            key = "A_ost2"
                        qk_chunk(j, st, fc, bd64, "bd64", [(tabs[:, T_DKG:T_DKG + 1], dst, key)], xnb, col0, rr=(None, 4, 16)[g])
            for cc in range(2):
                fc = 20 + cc
                qk_chunk(j, st, fc, bd32, "bd32", [(tabs[:, T_FKG:T_FKG + 1], ost[:, 12 + cc, :], okey)], xnb, col0)
            if own:
                t0 = (st - 2) * 2048 + jl * 512
                P.dma(d["qd"][0:512, t0:t0 + 512].rearrange("(c p) t -> p c t", p=128), ost[:, 0:4, :], reads=[okey])
                P.dma(d["qf1"][:, t0:t0 + 512].rearrange("(c p) t -> p c t", p=128), ost[:, 8:10, :], reads=[okey])
                P.dma(d["qf2"][:, t0:t0 + 512].rearrange("(c p) t -> p c t", p=128), ost[:, 10:12, :], reads=[okey])
            if need_dil:
                t1 = (st - 1) * 2048 + jl * 512
                P.dma(d["kd"][0:512, t1:t1 + 512].rearrange("(c p) t -> p c t", p=128), ost[:, 4:8, :], reads=[okey])
            P.dma(d["kf"][:, j * 512:(j + 1) * 512].rearrange("(c p) t -> p c t", p=128), ost[:, 12:14, :], reads=[okey])
        if own:
            t0 = (st - 2) * 2048
            P.dma(d["qd"][512:768, t0:t0 + 2048].rearrange("(c p) t -> p c t", p=128), ost2[:, 0:2, :], reads=["A_ost2"])
        if need_dil:
            t1 = (st - 1) * 2048
            P.dma(d["kd"][512:768, t1:t1 + 2048].rearrange("(c p) t -> p c t", p=128), ost2[:, 2:4, :], reads=["A_ost2"])
        xkeys = ["A_xn_%d_%d" % (q_, c) for c in range(8) for q_ in range(4)]
        if need_dil:
            if st == 1:
                P.add("pool", lambda e: e.tensor_copy(out=vst[:, :, :, 64:65], in_=tabs[:, T_PV:T_PV + 1].unsqueeze(1).unsqueeze(1).to_broadcast([128, 12, 16, 1])),
                      reads=["tabs"], writes=["A_vst"])
            elif st == 2:
                P.add("pool", lambda e: e.memset(vst[:, :, :, 64:65], 1.0), writes=["A_vst"])
            for g in range(3):
                d_ = DILS[g]
                nsp = 2048 // (128 * d_)
                for sp in range(nsp):
                    for r in range(d_):
                        blk = sp * d_ + r
                        pi = cnt["pj"] % 4
                        cnt["pj"] += 1
                        pk = "A_pj%d" % pi
                        s0 = sp * 128 * d_ + r
                        for c in range(8):
                            P.add("pe", lambda e, c=c, s0=s0, d_=d_, g=g, pi=pi: e.matmul(
                                pj[pi][:, 0:256], lhsT=xnb[:, c, s0:s0 + 127 * d_ + 1:d_],
                                rhs=w[:, c, 1536 + g * 256:1536 + (g + 1) * 256], start=(c == 0), stop=(c == 7)),
                                reads=["A_w"] + xkeys, writes=[pk])
                        eng = "act" if blk % 2 == 0 else "dve"
                        if eng == "act":
                            P.add("act", lambda e, g=g, blk=blk, pi=pi: e.activation(
                                out=vst[:, g * 4:(g + 1) * 4, blk, 0:64],
                                in_=pj[pi][:, 0:256].rearrange("p (h f) -> p h f", h=4), func=AF.Copy),
                                reads=[pk], writes=["A_vst"])
                        else:
                            P.add("dve", lambda e, g=g, blk=blk, pi=pi: e.tensor_copy(
                                out=vst[:, g * 4:(g + 1) * 4, blk, 0:64],
                                in_=pj[pi][:, 0:256].rearrange("p (h f) -> p h f", h=4)),
                                reads=[pk], writes=["A_vst"])
            P.dma(d["vd"][:, :, (st - 1) * 16:st * 16, :].rearrange("h p b f -> p h b f"), vst[:], reads=["A_vst"])
        src_f = facv if st < 2 else fac
        P.add("pool", lambda e, src_f=src_f: e.tensor_copy(out=vfst[:, :, :, 64:65],
                                                          in_=src_f[:].unsqueeze(2).unsqueeze(3).to_broadcast([128, 4, 16, 1])),
              reads=["A_fac", "A_facv"], writes=["A_vfst"])
        for bk in range(16):
            pi = cnt["pj"] % 4
            cnt["pj"] += 1
            pk = "A_pj%d" % pi
            for c in range(8):
                P.add("pe", lambda e, c=c, bk=bk, pi=pi: e.matmul(pj[pi][:, 0:256], lhsT=xnb[:, c, bk * 128:(bk + 1) * 128],
                                                                   rhs=w[:, c, 2816:3072], start=(c == 0), stop=(c == 7)),
                      reads=["A_w"] + xkeys, writes=[pk])
            P.add("dve", lambda e, bk=bk, pi=pi: e.tensor_tensor(out=vfst[:, :, bk, 0:64],
                                                                in0=pj[pi][:, 0:256].rearrange("p (h f) -> p h f", h=4),
                                                                in1=fac256[:], op=ALU.mult),
                  reads=[pk, "A_fac256"], writes=["A_vfst"])
        P.dma(d["vf"][:, :, st * 16:(st + 1) * 16, :].rearrange("h p b f -> p h b f"), vfst[:], reads=["A_vfst"])


def phase_B(P, C, d):
    tabs = C.tabs
    mixT = d["mixT"]
    onesf = P.sb("B_onesf", [128, 128], F32)
    P.add("pool", lambda e: e.memset(onesf[:], 1.0), writes=["B_onesf"])
    ones64 = C.blockdiag(64, 1.0 / 64, "bd64")
    Mt = P.sb("B_M", [128, 12, 256], BF16)
    stp = P.sb("B_stp", [128, 256], F32)
    mtmp = P.sb("B_mtmp", [128, 256], F32)
    P.add("pool", lambda e: e.iota(stp[:, 0:128], pattern=[[1, 128]], base=128, channel_multiplier=-1,
                                   allow_small_or_imprecise_dtypes=True), writes=["B_stp"])
    P.add("pool", lambda e: e.iota(stp[:, 128:256], pattern=[[1, 128]], base=0, channel_multiplier=-1,
                                   allow_small_or_imprecise_dtypes=True), writes=["B_stp"])
    for h in range(12):
        sc = -SLOPES[h] * DILS[h // 4]
        P.add("dve", lambda e: e.tensor_scalar(out=mtmp[:], in0=stp[:], scalar1=0.0, scalar2=0.0, op0=ALU.max, op1=ALU.add),
              reads=["B_stp"], writes=["B_mtmp"])
        P.add("act", lambda e, sc=sc: e.activation(out=mtmp[:], in_=mtmp[:], func=AF.Exp, scale=sc), reads=["B_mtmp"], writes=["B_mtmp"])
        P.add("pool", lambda e: e.affine_select(out=mtmp[:, 0:128], in_=mtmp[:, 0:128], pattern=[[-1, 128]], compare_op=ALU.is_ge,
                                                fill=0.0, base=0, channel_multiplier=1), reads=["B_mtmp"], writes=["B_mtmp"])
        P.add("pool", lambda e: e.affine_select(out=mtmp[:, 128:256], in_=mtmp[:, 128:256], pattern=[[1, 128]], compare_op=ALU.is_ge,
                                                fill=0.0, base=0, channel_multiplier=-1), reads=["B_mtmp"], writes=["B_mtmp"])
        P.add("dve", lambda e, h=h: e.tensor_copy(out=Mt[:, h, :], in_=mtmp[:]), reads=["B_mtmp"], writes=["B_M"])
    cm = P.sb("B_cm", [128, 4, 512], BF16)
    P.add("pool", lambda e: e.memset(cm[:], 1.0), writes=["B_cm"])
    for i in range(4):
        P.add("pool", lambda e, i=i: e.affine_select(out=cm[:, i, :], in_=cm[:, i, :], pattern=[[1, 512]], compare_op=ALU.is_ge,
                                                     fill=0.0, base=-128 * i, channel_multiplier=-1), reads=["B_cm"], writes=["B_cm"])
    lp = P.sb("B_lp", [128, 2], F32)
    P.add("pool", lambda e: e.memset(lp[:], 0.0), writes=["B_lp"])
    P.add("dve", lambda e: e.tensor_tensor(out=lp[0:32, 0:1], in0=tabs[0:32, T_LQ1:T_LQ1 + 1], in1=tabs[0:32, T_LQ1 + 1:T_LQ1 + 2], op=ALU.mult),
          reads=["tabs", "B_lp"], writes=["B_lp"])
    P.add("dve", lambda e: e.tensor_tensor(out=lp[0:32, 1:2], in0=tabs[0:32, T_LQ1 + 2:T_LQ1 + 3], in1=tabs[0:32, T_LQ1 + 3:T_LQ1 + 4], op=ALU.mult),
          reads=["tabs", "B_lp"], writes=["B_lp"])
    bank = [P.ps("B_bank%d" % i, [128, 1024]) for i in range(4)]
    bk = ["B_bank%d" % i for i in range(4)]
    P.add("pe", lambda e: e.matmul(bank[0][:, 0:2], lhsT=onesf[:, :], rhs=lp[:, :], start=True, stop=True),
          reads=["B_onesf", "B_lp"], writes=[bk[0]])
    le = P.sb("B_le", [128, 2], F32)
    neglam = P.sb("B_neglam", [128, 1], F32)
    g08 = P.sb("B_g08", [128, 1], F32)
    P.add("act", lambda e: e.activation(out=le[:], in_=bank[0][:, 0:2], func=AF.Exp), reads=[bk[0]], writes=["B_le"])
    P.add("dve", lambda e: e.tensor_tensor(out=neglam[:], in0=le[:, 1:2], in1=le[:, 0:1], op=ALU.subtract), reads=["B_le"], writes=["B_neglam"])
    P.add("dve", lambda e: e.tensor_scalar(out=neglam[:], in0=neglam[:], scalar1=-0.2, scalar2=0.0, op0=ALU.add, op1=ALU.add),
          reads=["B_neglam"], writes=["B_neglam"])
    P.add("dve", lambda e: e.tensor_scalar(out=g08[:], in0=tabs[:, T_DOG:T_DOG + 1], scalar1=0.8, scalar2=0.0, op0=ALU.mult, op1=ALU.add),
          reads=["tabs"], writes=["B_g08"])

    qb = [P.sb("B_q%d" % i, [128, 3, 2048], BF16) for i in range(1)]
    kb_ = [P.sb("B_k%d" % i, [128, 3, 4096], BF16) for i in range(1)]
    vb = [P.sb("B_v%d" % i, [128, 3, 32, 65], BF16) for i in range(1)]
    nd = P.sb("B_nd", [65, 3, 2048], F32)
    Eb = [P.sb("B_E%d" % i, [128, 256], BF16) for i in range(3)]
    Pb = [P.sb("B_P%d" % i, [128, 256], BF16) for i in range(3)]
    rinv = P.sb("B_rinv", [64, 512], F32)
    mixst = P.sb("B_mixst", [64, 3, 2048], BF16)
    unit = 0
    cnt = 0
    for hp in range(4):
        for S in range(2):
            bi = 0
            unit += 1
            hf_ = (hp % 2) * 64
            P.add("pool", lambda e, bi=bi: e.memset(qb[bi][:], 0.0), writes=["B_q%d_%d" % (bi, g) for g in range(3)])
            for g in range(3):
                r0 = (4 * g + hp) * 64
                rp = (4 * g + (hp // 2) * 2) * 64
                P.dma(qb[bi][hf_:hf_ + 64, g, :], d["qd"][r0:r0 + 64, S * 2048:(S + 1) * 2048], writes=["B_q%d_%d" % (bi, g)])
                P.dma(kb_[bi][:, g, :], d["kd"][rp:rp + 128, S * 2048:S * 2048 + 4096], writes=["B_k%d_%d" % (bi, g)])
                P.dma(vb[bi][:, g, :, :], d["vd"][4 * g + hp, :, S * 16:S * 16 + 32, :], writes=["B_v%d_%d" % (bi, g)])
            items = []
            for g in range(3):
                dd = DILS[g]
                h = 4 * g + hp
                for blk in range(16):
                    sp, r = blk // dd, blk % dd
                    pb_, cb_ = 16 + blk - dd, 16 + blk
                    ps = bank[cnt % 2]
                    psk = bk[cnt % 2]
                    E = Eb[cnt % 3]
                    Pm = Pb[cnt % 3]
                    ek, pk_ = "B_E%d" % (cnt % 3), "B_P%d" % (cnt % 3)
                    po = bank[2 + cnt % 2]
                    pok = bk[2 + cnt % 2]
                    cnt += 1
                    par = cnt % 2
                    qs = qb[bi][:, g, blk * 128:(blk + 1) * 128]

                    def st1(ps=ps, psk=psk, g=g, pb_=pb_, cb_=cb_, qs=qs, bi=bi):
                        P.add("pe", lambda e: e.matmul(ps[:, 0:128], lhsT=kb_[bi][:, g, pb_ * 128:(pb_ + 1) * 128], rhs=qs, start=True, stop=True),
                              reads=["B_q%d_%d" % (bi, g), "B_k%d_%d" % (bi, g)], writes=[psk])
                        P.add("pe", lambda e: e.matmul(ps[:, 128:256], lhsT=kb_[bi][:, g, cb_ * 128:(cb_ + 1) * 128], rhs=qs, start=True, stop=True),
                              reads=["B_q%d_%d" % (bi, g), "B_k%d_%d" % (bi, g)], writes=[psk])

                    def st2(ps=ps, psk=psk, E=E, Pm=Pm, ek=ek, pk_=pk_, h=h, par=par):
                        P.add("act", lambda e: e.activation(out=E[:], in_=ps[:, 0:256], func=AF.Exp, scale=0.125), reads=[psk], writes=[ek])
                        eng = "pool" if par == 0 else "dve"
                        P.add(eng, lambda e: e.tensor_tensor(out=Pm[:], in0=E[:], in1=Mt[:, h, :], op=ALU.mult), reads=[ek, "B_M"], writes=[pk_])

                    def st3(po=po, pok=pok, Pm=Pm, pk_=pk_, g=g, pb_=pb_, cb_=cb_, bi=bi, sp=sp, r=r, dd=dd, par=par):
                        P.add("pe", lambda e: e.matmul(po[0:65, 0:128], lhsT=vb[bi][:, g, pb_, :], rhs=Pm[:, 0:128], start=True, stop=False),
                              reads=[pk_, "B_v%d_%d" % (bi, g)], writes=[pok])
                        P.add("pe", lambda e: e.matmul(po[0:65, 0:128], lhsT=vb[bi][:, g, cb_, :], rhs=Pm[:, 128:256], start=False, stop=True),
                              reads=[pk_, "B_v%d_%d" % (bi, g)], writes=[pok])
                        t0 = sp * 128 * dd + r
                        dst = nd[:, g, t0:t0 + 127 * dd + 1:dd]
                        if par == 0:
                            P.add("act", lambda e: e.activation(out=dst, in_=po[0:65, 0:128], func=AF.Copy), reads=[pok], writes=["B_nd%d" % g])
                        else:
                            P.add("dve", lambda e: e.tensor_copy(out=dst, in_=po[0:65, 0:128]), reads=[pok], writes=["B_nd%d" % g])
                    items.append((st1, st2, st3))
            items[0][0]()
            for ii in range(len(items)):
                if ii + 1 < len(items):
                    items[ii + 1][0]()
                items[ii][1]()
                items[ii][2]()
            for cc in range(4):
                pd = bank[cnt % 2]
                pdk = bk[cnt % 2]
                cnt += 1
                for g in range(3):
                    P.add("pe", lambda e, pd=pd, g=g, cc=cc: e.matmul(pd[0:64, 0:512], lhsT=onesf[64:65, 0:64], rhs=nd[64:65, g, cc * 512:(cc + 1) * 512],
                                                                     start=(g == 0), stop=(g == 2)), reads=["B_onesf", "B_nd%d" % g], writes=[pdk])
                P.add("dve", lambda e, pd=pd: e.reciprocal(out=rinv[:], in_=pd[0:64, 0:512]), reads=[pdk], writes=["B_rinv"])
                for g in range(3):
                    eng = "pool" if g == 1 else "dve"
                    P.add(eng, lambda e, g=g, cc=cc: e.tensor_tensor(out=mixst[:, g, cc * 512:(cc + 1) * 512], in0=nd[0:64, g, cc * 512:(cc + 1) * 512],
                                                                    in1=rinv[:], op=ALU.mult), reads=["B_nd%d" % g, "B_rinv"], writes=["B_mixst%d" % g])
            for g in range(3):
                r0 = (4 * g + hp) * 64
                P.dma(mixT[r0:r0 + 64, S * 2048:(S + 1) * 2048], mixst[:, g, :], reads=["B_mixst%d" % g])

    kf_ = P.sb("B_kf", [128, 8192], BF16)
    vf_ = P.sb("B_vf", [128, 64, 65], BF16)
    q12 = P.sb("B_q12", [128, 2, 4096], BF16)
    E2 = [P.sb("B_E2_%d" % i, [128, 1024], BF16) for i in range(3)]
    dn = P.sb("B_dn", [65, 1024], F32)
    rb = P.sb("B_rb", [64, 1024], F32)
    t1 = P.sb("B_t1", [64, 512], F32)
    t2 = P.sb("B_t2", [64, 512], F32)
    sqd = P.sb("B_sqd", [64, 512], BF16)
    sdd = P.sb("B_sdd", [64, 512], F32)
    mixf = [P.sb("B_mixf%d" % i, [64, 512], BF16) for i in range(2)]
    sc32 = 32.0 ** -0.5
    ec = 0
    for h in range(4):
        sl = SLOPES[12 + h]
        hf_ = (h % 2) * 64
        rp = (h // 2) * 128
        P.dma(kf_[:], d["kf"][rp:rp + 128, :], writes=["B_kf"])
        P.add("pool", lambda e: e.memset(q12[:], 0.0), writes=["B_q12"])
        P.dma(vf_[:], d["vf"][h], writes=["B_vf"])
        P.dma(q12[hf_:hf_ + 64, 0, :], d["qf1"][h * 64:(h + 1) * 64, :], writes=["B_q12"])
        P.dma(q12[hf_:hf_ + 64, 1, :], d["qf2"][h * 64:(h + 1) * 64, :], writes=["B_q12"])
        for jj in range(8):
            nkb = 32 + 4 * jj + 4
            po = bank[2]
            pok = bk[2]
            items = []
            for kb in range(nkb):
                ps = bank[kb % 2]
                psk = bk[kb % 2]
                E = E2[ec % 3]
                ek = "B_E2_%d" % (ec % 3)
                ec += 1
                off = -sl * (4096 + jj * 512 - kb * 128)
                di = kb - (32 + 4 * jj)

                def st1(ps=ps, psk=psk, kb=kb, jj=jj):
                    for m in range(2):
                        P.add("pe", lambda e, m=m: e.matmul(ps[:, m * 512:(m + 1) * 512], lhsT=kf_[:, kb * 128:(kb + 1) * 128],
                                                            rhs=q12[:, m, jj * 512:(jj + 1) * 512], start=True, stop=True),
                              reads=["B_kf", "B_q12"], writes=[psk])

                def st2(ps=ps, psk=psk, E=E, ek=ek, off=off, di=di):
                    P.add("act", lambda e: e.activation(out=E[:], in_=ps[:], func=AF.Exp, bias=off, scale=sc32), reads=[psk], writes=[ek])
                    if di >= 0:
                        eng = "pool" if di % 2 == 0 else "dve"
                        P.add(eng, lambda e: e.tensor_tensor(out=E[:].rearrange("p (m q) -> p m q", m=2), in0=E[:].rearrange("p (m q) -> p m q", m=2),
                                                             in1=cm[:, di, :].unsqueeze(1).to_broadcast([128, 2, 512]), op=ALU.mult),
                              reads=[ek, "B_cm"], writes=[ek])

                def st3(E=E, ek=ek, kb=kb, nkb=nkb):
                    for m in range(2):
                        P.add("pe", lambda e, m=m: e.matmul(po[0:65, m * 512:(m + 1) * 512], lhsT=vf_[:, kb, :], rhs=E[:, m * 512:(m + 1) * 512],
                                                            start=(kb == 0), stop=(kb == nkb - 1)), reads=[ek, "B_vf"], writes=[pok])
                items.append((st1, st2, st3))
            items[0][0]()
            for ii in range(len(items)):
                if ii + 1 < len(items):
                    items[ii + 1][0]()
                items[ii][1]()
                items[ii][2]()
            P.add("act", lambda e: e.activation(out=dn[64:65, :], in_=po[64:65, :], func=AF.Copy), reads=[pok], writes=["B_dn"])
            P.add("dve", lambda e: e.reciprocal(out=dn[64:65, :], in_=dn[64:65, :]), reads=["B_dn"], writes=["B_dn"])
            pbb = bank[3]
            for m in range(2):
                P.add("pe", lambda e, m=m: e.matmul(pbb[0:64, m * 512:(m + 1) * 512], lhsT=onesf[64:65, 0:64], rhs=dn[64:65, m * 512:(m + 1) * 512], start=True, stop=True),
                      reads=["B_dn", "B_onesf"], writes=[bk[3]])
            P.add("act", lambda e: e.activation(out=rb[:], in_=pbb[0:64, :], func=AF.Copy), reads=[bk[3]], writes=["B_rb"])
            P.add("dve", lambda e: e.tensor_tensor(out=t1[:], in0=po[0:64, 0:512], in1=rb[:, 0:512], op=ALU.mult), reads=[pok, "B_rb"], writes=["B_t1"])
            P.add("dve", lambda e: stt(e, t2[:], po[0:64, 512:1024], neglam[0:64, 0:1], rb[:, 512:1024]), reads=[pok, "B_rb", "B_neglam"], writes=["B_t2"])
            P.add("pool", lambda e: e.tensor_tensor(out=t1[:], in0=t1[:], in1=t2[:], op=ALU.add), reads=["B_t1", "B_t2"], writes=["B_t1"])
            P.add("act", lambda e: e.activation(out=sqd[:], in_=t1[:], func=AF.Square), reads=["B_t1"], writes=["B_sqd"])
            P.add("pe", lambda e: e.matmul(pbb[0:64, 0:512], lhsT=ones64[0:64, 0:64], rhs=sqd[:], start=True, stop=True), reads=["B_sqd", "bd_bd64"], writes=[bk[3]])
            P.add("act", lambda e: e.activation(out=sdd[:], in_=pbb[0:64, 0:512], func=AF.Sqrt, bias=EPS, scale=1.0), reads=[bk[3]], writes=["B_sdd"])
            P.add("dve", lambda e: e.reciprocal(out=sdd[:], in_=sdd[:]), reads=["B_sdd"], writes=["B_sdd"])
            mf = mixf[jj % 2]
            mk = "B_mixf%d" % (jj % 2)
            P.add("dve", lambda e, mf=mf: stt(e, mf[:], t1[:], g08[0:64, 0:1], sdd[:]), reads=["B_t1", "B_sdd", "B_g08"], writes=[mk])
            r0 = 768 + h * 64
            P.dma(mixT[r0:r0 + 64, jj * 512:(jj + 1) * 512], mf[:], reads=[mk])


def load_w_bf16(P, name, dram2d, K, N):
    kc = K // 128
    w = P.sb(name, [128, kc, N], BF16)
    for c in range(kc):
        P.dma(w[:, c, :], dram2d[c * 128:(c + 1) * 128, :], writes=[name], q="pool")
    return w


def phase_C(P, C, d):
    tabs = C.tabs
    N = 256
    stg = P.sb("C_stg", [128, 4, 512], F32)
    slots = [stg[:, i, :] for i in range(4)]
    skeys = ["C_stg%d" % i for i in range(4)]
    wout = load_w_hw(P, "C_wout", d["attn_w_out"], 1024, 1024, slots, skeys)
    wg = load_w_hw(P, "C_wg", d["ffn_w_gate"], 1024, 2816, slots, skeys)
    wu = load_w_hw(P, "C_wu", d["ffn_w_up"], 1024, 2816, slots, skeys)
    wd = load_w_hw(P, "C_wd", d["ffn_w_down"], 2816, 1024, slots, skeys)
    mix = [P.sb("C_mix%d" % i, [128, 8, N], BF16) for i in range(2)]
    xh = [P.sb("C_xh%d" % i, [128, 8, N], F32) for i in range(2)]
    sq = P.sb("C_sq", [128, 8, N], BF16)
    sd = P.sb("C_sd", [128, N], F32)
    rstd = P.sb("C_rstd", [128, N], F32)
    xn = P.sb("C_xn", [128, 8, N], BF16)
    a = P.sb("C_a", [128, 22, N], BF16)
    sg = [P.sb("C_sg%d" % i, [128, N], F32) for i in range(2)]
    ps_ss = P.ps("C_ps_ss", [128, 512])
    pb = [P.ps("C_pb%d" % i, [128, 512]) for i in range(6)]
    xTv = d["xT"].rearrange("(c p) t -> p c t", p=128)
    mTv = d["mixT"].rearrange("(c p) t -> p c t", p=128)
    hTv = d["h2T"].rearrange("(c p) t -> p c t", p=128)
    pc = 0
    pend = [None]
    for j in range(4096 // N):
        b = j % 2
        P.dma(mix[b][:], mTv[:, :, j * N:(j + 1) * N], writes=["C_mix%d" % b])
        P.dma(xh[b][:], xTv[:, :, 4096 + j * N:4096 + (j + 1) * N], writes=["C_xh%d_%d" % (b, c) for c in range(8)])
        for oc in range(8):
            pp = pb[pc % 6]; pk = "C_pb%d" % (pc % 6); pc += 1
            for c in range(8):
                P.add("pe", lambda e, pp=pp, c=c, oc=oc, b=b: e.matmul(pp[:, 0:N], lhsT=wout[:, c, oc * 128:(oc + 1) * 128], rhs=mix[b][:, c, :],
                                                                       start=(c == 0), stop=(c == 7)), reads=["C_wout_cb%d" % (oc // 4), "C_mix%d" % b], writes=[pk])
            P.add("dve", lambda e, pp=pp, oc=oc, b=b: e.tensor_tensor(out=xh[b][:, oc, :], in0=pp[:, 0:N], in1=xh[b][:, oc, :], op=ALU.add),
                  reads=[pk, "C_xh%d_%d" % (b, oc)], writes=["C_xh%d_%d" % (b, oc)])
        xkeys = ["C_xh%d_%d" % (b, c) for c in range(8)]
        onesm = C.ones_scaled(1.0 / 1024, "d1024")
        P.add("act", lambda e, b=b: e.activation(out=sq[:], in_=xh[b][:], func=AF.Square), reads=xkeys, writes=["C_sq"])
        for c in range(8):
            P.add("pe", lambda e, c=c: e.matmul(ps_ss[:, 0:N], lhsT=onesm[:], rhs=sq[:, c, :], start=(c == 0), stop=(c == 7)),
                  reads=["C_sq", "ones_d1024"], writes=["C_ps_ss"])
        P.add("act", lambda e: e.activation(out=sd[:], in_=ps_ss[:, 0:N], func=AF.Sqrt, bias=EPS, scale=1.0), reads=["C_ps_ss"], writes=["C_sd"])
        P.add("dve", lambda e: e.reciprocal(out=rstd[:], in_=sd[:]), reads=["C_sd"], writes=["C_rstd"])
        for c in range(8):
            P.add("dve", lambda e, c=c, b=b: stt(e, xn[:, c, :], xh[b][:, c, :], tabs[:, T_FFN_NORM + c:T_FFN_NORM + c + 1], rstd[:]),
                  reads=["C_xh%d_%d" % (b, c), "C_rstd", "tabs"], writes=["C_xn%d" % c])
        nkeys = ["C_xn%d" % c for c in range(8)]
        if pend[0] is not None:
            pend[0]()
            pend[0] = None
        for f in range(22):
            pg = pb[pc % 6]; pgk = "C_pb%d" % (pc % 6); pc += 1
            pu = pb[pc % 6]; puk = "C_pb%d" % (pc % 6); pc += 1
            for c in range(8):
                P.add("pe", lambda e, pg=pg, c=c, f=f: e.matmul(pg[:, 0:N], lhsT=wg[:, c, f * 128:(f + 1) * 128], rhs=xn[:, c, :], start=(c == 0), stop=(c == 7)),
                      reads=["C_wg_cb%d" % (f // 4)] + nkeys, writes=[pgk])
            for c in range(8):
                P.add("pe", lambda e, pu=pu, c=c, f=f: e.matmul(pu[:, 0:N], lhsT=wu[:, c, f * 128:(f + 1) * 128], rhs=xn[:, c, :], start=(c == 0), stop=(c == 7)),
                      reads=["C_wu_cb%d" % (f // 4)] + nkeys, writes=[puk])
            s_ = sg[f % 2]; sk = "C_sg%d" % (f % 2)
            P.add("act", lambda e, pg=pg, s_=s_: e.activation(out=s_[:], in_=pg[:, 0:N], func=AF.Silu), reads=[pgk], writes=[sk])
            P.add("dve", lambda e, pu=pu, s_=s_, f=f: e.tensor_tensor(out=a[:, f, :], in0=pu[:, 0:N], in1=s_[:], op=ALU.mult), reads=[puk, sk], writes=["C_a%d" % f])
        akeys = ["C_a%d" % f for f in range(22)]

        def down_store(j=j, b=b, xkeys=xkeys, akeys=akeys):
            nonlocal pc
            for oc in range(8):
                pp = pb[pc % 6]; pk = "C_pb%d" % (pc % 6); pc += 1
                for f in range(22):
                    P.add("pe", lambda e, pp=pp, f=f, oc=oc: e.matmul(pp[:, 0:N], lhsT=wd[:, f, oc * 128:(oc + 1) * 128], rhs=a[:, f, :], start=(f == 0), stop=(f == 21)),
                          reads=["C_wd_cb%d" % (oc // 4)] + akeys, writes=[pk])
                P.add("dve", lambda e, pp=pp, oc=oc: e.tensor_tensor(out=xh[b][:, oc, :], in0=pp[:, 0:N], in1=xh[b][:, oc, :], op=ALU.add),
                      reads=[pk, "C_xh%d_%d" % (b, oc)], writes=["C_xh%d_%d" % (b, oc)])
            P.dma(hTv[:, :, j * N:(j + 1) * N], xh[b][:], reads=xkeys)
        pend[0] = down_store
    pend[0]()


def phase_D(P, C, d, zero_init=False, scale_pv=False, write_h=True, state_only=False):
    tabs = C.tabs
    ident = C.ident
    N = 512
    full = not state_only
    ht = P.sb("D_ht", [128, 8, N], F32)
    hslots = [ht[:, i, :] for i in range(8)]
    hslot_keys = ["D_ht%d" % i for i in range(8)]
    if full:
        w = load_w_hw(P, "D_w", d["hgrn_w_in"], 1024, 4096, hslots, hslot_keys, keyfn=lambda c, col0: "D_w")
        wo = load_w_hw(P, "D_wo", d["hgrn_w_out"], 1024, 1024, hslots, hslot_keys, keyfn=lambda c, col0: "D_wo")
    else:
        w = P.sb("D_w", [128, 8, 4096], BF16)
        wpart = load_w_hw(P, "D_wfi", d["hgrn_w_in"][:, 1024:3072], 1024, 2048, hslots, hslot_keys, keyfn=lambda c, col0: "D_w")
        w_full = w
        class _W:
            def __getitem__(self, key):
                p_, c_, cols = key
                return wpart[p_, c_, slice(cols.start - 1024, cols.stop - 1024)]
        w = _W()
    onesm = C.ones_scaled(1.0 / 1024, "d1024")
    ones128 = C.ones_scaled(1.0 / 128, "d128")
    lb = P.sb("D_lb", [128, 8], F32)
    oml = P.sb("D_oml", [128, 8], F32)
    P.add("dve", lambda e: e.tensor_tensor(out=lb[:], in0=tabs[:, T_LB0:T_LB0 + 8], in1=tabs[:, T_LB1:T_LB1 + 8], op=ALU.subtract), reads=["tabs"], writes=["D_lb"])
    P.add("act", lambda e: e.activation(out=lb[:], in_=lb[:], func=AF.Exp), reads=["D_lb"], writes=["D_lb"])
    P.add("dve", lambda e: e.tensor_scalar(out=lb[:], in0=lb[:], scalar1=1.0, scalar2=0.0, op0=ALU.add, op1=ALU.add), reads=["D_lb"], writes=["D_lb"])
    P.add("dve", lambda e: e.reciprocal(out=lb[:], in_=lb[:]), reads=["D_lb"], writes=["D_lb"])
    P.add("dve", lambda e: e.tensor_scalar(out=oml[:], in0=lb[:], scalar1=-1.0, scalar2=1.0, op0=ALU.mult, op1=ALU.add), reads=["D_lb"], writes=["D_oml"])
    noml = P.sb("D_noml", [128, 8], F32)
    P.add("dve", lambda e: e.tensor_scalar(out=noml[:], in0=oml[:], scalar1=-1.0, scalar2=0.0, op0=ALU.mult, op1=ALU.add), reads=["D_oml"], writes=["D_noml"])
    m2 = P.sb("D_m2", [128, 128], F32)
    P.add("pool", lambda e: e.memset(m2[:], 1.0), writes=["D_m2"])
    P.add("pool", lambda e: e.affine_select(out=m2[:], in_=m2[:], pattern=[[1, 128]], compare_op=ALU.is_ge, fill=0.0, base=0, channel_multiplier=-1),
          reads=["D_m2"], writes=["D_m2"])
    P.add("pool", lambda e: e.memset(m2[0:64, 64:128], 0.0), reads=["D_m2"], writes=["D_m2"])
    rm = P.sb("D_rm", [128, N], F32)
    P.add("pool", lambda e: e.memset(rm[:], 1.0), writes=["D_rm"])
    P.add("pool", lambda e: e.memset(rm[:].rearrange("p (a b) -> p a b", b=64)[:, :, 0:1], 0.0), reads=["D_rm"], writes=["D_rm"])
    stf = P.sb("D_stf", [128, 8, 128], F32)
    stb = P.sb("D_stb", [128, 8, 128], BF16)
    skeys = ["D_stf%d" % h for h in range(8)]
    if zero_init:
        P.add("pool", lambda e: e.memset(stf[:], 0.0), writes=skeys)
    else:
        P.dma(stf[:], d["s_in"].rearrange("h k v -> k h v"), writes=skeys)
        if scale_pv:
            P.add("dve", lambda e: e.tensor_scalar(out=stf[:], in0=stf[:], scalar1=tabs[:, T_PV:T_PV + 1], scalar2=0.0, op0=ALU.mult, op1=ALU.add),
                  reads=["tabs"] + skeys, writes=skeys)
    if full:
        for h in range(8):
            P.add("act", lambda e, h=h: e.activation(out=stb[:, h, :], in_=stf[:, h, :], func=AF.Copy), reads=["D_stf%d" % h], writes=["D_stb%d" % h])
    sqg = P.sb("D_sqg", [128, 8, N], BF16)
    sq = sqg
    gated = sqg
    xn = P.sb("D_xn", [128, 8, N], BF16)
    Vt = P.sb("D_Vt", [128, 4, 1024], BF16)
    NB = 2

    def mk(nm, dt=F32, n=NB):
        return [P.sb("D_%s%d" % (nm, i), [128, N], dt) for i in range(n)]
    t_f, t_kk, t_b, t_d1, t_d2, t_x = mk("f"), mk("kk"), mk("b"), mk("d1"), mk("d2"), mk("x")
    t_Qt, t_Kt, t_ks = mk("Qt", BF16), mk("Kt", BF16), mk("ks", BF16)
    sd, rstd = t_x[0], t_d1[0]
    p_ksT = mk("ksT", BF16, 8)
    decs = P.sb("D_decs", [128, 8, 8], F32)
    if full:
        p_qi, p_At, p_sg = mk("qi", BF16, 8), mk("At", BF16, 8), mk("sg", BF16, 8)
        p_o = mk("o", F32, 8)
    pj = [P.ps("D_pj%d" % i, [128, 512]) for i in range(2)]
    psS = P.ps("D_psS", [128, 512])
    psT = P.ps("D_psT", [128, 512], BF16)
    pob = [P.ps("D_po%d" % i, [128, 512]) for i in range(2)]
    pkvb = [P.ps("D_pkv%d" % i, [128, 512]) for i in range(2)]
    hv = d["h2T"].rearrange("(c p) t -> p c t", p=128)
    ov = d["h3T"].rearrange("(c p) t -> p c t", p=128)
    pc = 0
    hc = 0
    kvc = 0
    poc = 0
    for j in range(4096 // N):
        P.dma(ht[:], hv[:, :, j * N:(j + 1) * N], writes=["D_ht%d" % c for c in range(8)])
        hkeys = ["D_ht%d" % c for c in range(8)]
        P.add("act", lambda e: e.activation(out=sq[:], in_=ht[:], func=AF.Square), reads=hkeys, writes=["D_sq"])
        gen = pj[pc % 2]; gk = "D_pj%d" % (pc % 2); pc += 1
        for c in range(8):
            P.add("pe", lambda e, c=c, gen=gen: e.matmul(gen[:, :], lhsT=onesm[:], rhs=sq[:, c, :], start=(c == 0), stop=(c == 7)), reads=["D_sq", "ones_d1024"], writes=[gk])
        P.add("act", lambda e, gen=gen: e.activation(out=sd[:], in_=gen[:], func=AF.Sqrt, bias=EPS, scale=1.0), reads=[gk], writes=["D_x0"])
        P.add("dve", lambda e: e.reciprocal(out=rstd[:], in_=sd[:]), reads=["D_x0"], writes=["D_d10"])
        for c in range(8):
            P.add("dve", lambda e, c=c: stt(e, xn[:, c, :], ht[:, c, :], tabs[:, T_HGRN_NORM + c:T_HGRN_NORM + c + 1], rstd[:]),
                  reads=["D_ht%d" % c, "D_d10", "tabs"], writes=["D_xn%d" % c])
        nkeys = ["D_xn%d" % c for c in range(8)]
        for tb in range(4):
            for hf in range(2):
                gen = pj[pc % 2]; gk = "D_pj%d" % (pc % 2); pc += 1
                for c in range(8):
                    P.add("pe", lambda e, c=c, tb=tb, hf=hf, gen=gen: e.matmul(gen[:, :], lhsT=xn[:, c, tb * 128:(tb + 1) * 128], rhs=w[:, c, 2048 + hf * 512:2048 + (hf + 1) * 512],
                                                                       start=(c == 0), stop=(c == 7)), reads=["D_w"] + nkeys, writes=[gk])
                if (tb + hf) % 2 == 0:
                    P.add("act", lambda e, tb=tb, hf=hf, gen=gen: e.activation(out=Vt[:, tb, hf * 512:(hf + 1) * 512], in_=gen[:], func=AF.Copy), reads=[gk], writes=["D_Vt"])
                else:
                    P.add("dve", lambda e, tb=tb, hf=hf, gen=gen: e.tensor_copy(out=Vt[:, tb, hf * 512:(hf + 1) * 512], in_=gen[:]), reads=[gk], writes=["D_Vt"])
        def head_prologue(h, bi):
            nonlocal pc
            K = lambda nm: "D_%s%d" % (nm, bi)
            KH = lambda nm: "D_%s%d" % (nm, h)
            f, kk, b, d1, d2, xx = t_f[bi], t_kk[bi], t_b[bi], t_d1[bi], t_d2[bi], t_x[bi]
            Qt, Kt, ks = t_Qt[bi], t_Kt[bi], t_ks[bi]
            ksT = p_ksT[h]

            def proj(col0):
                nonlocal pc
                pp = pj[pc % 2]; pk = "D_pj%d" % (pc % 2); pc += 1
                for c in range(8):
                    P.add("pe", lambda e, pp=pp, c=c, col0=col0: e.matmul(pp[:, :], lhsT=w[:, c, col0:col0 + 128], rhs=xn[:, c, :], start=(c == 0), stop=(c == 7)),
                          reads=["D_w"] + nkeys, writes=[pk])
                return pp, pk
            pf, pfk = proj(1024 + h * 128)
            P.add("act", lambda e: e.activation(out=f[:], in_=pf[:], func=AF.Sigmoid), reads=[pfk], writes=[K("f")])
            P.add("dve", lambda e: e.tensor_scalar(out=kk[:], in0=f[:], scalar1=noml[:, h:h + 1], scalar2=oml[:, h:h + 1], op0=ALU.mult, op1=ALU.add),
                  reads=[K("f"), "D_oml", "D_noml"], writes=[K("kk")])
            P.add("act", lambda e: e.activation(out=f[:], in_=f[:], func=AF.Ln, bias=lb[:, h:h + 1], scale=oml[:, h:h + 1]),
                  reads=[K("f"), K("kk"), "D_oml", "D_lb"], writes=[K("f")])
            P.add("dve", lambda e: e.tensor_tensor_scan(out=b[:], data0=rm[:], data1=f[:], initial=0.0, op0=ALU.mult, op1=ALU.add),
                  reads=[K("f"), "D_rm"], writes=[K("b")])
            b3 = b[:].rearrange("p (a b) -> p a b", b=64)
            P.add("pool", lambda e: e.tensor_tensor(out=d2[:].rearrange("p (a b) -> p a b", b=64), in0=b3[:, :, 63:64].to_broadcast([128, 8, 64]), in1=b3, op=ALU.subtract),
                  reads=[K("b")], writes=[K("d2")])
            if full:
                P.add("dve", lambda e: e.tensor_tensor(out=d1[:].rearrange("p (a b) -> p a b", b=64), in0=b3, in1=b3[:, :, 32:33].to_broadcast([128, 8, 64]), op=ALU.subtract),
                      reads=[K("b")], writes=[K("d1")])
            P.add("act", lambda e: e.activation(out=decs[:, h, :].unsqueeze(2), in_=b3[:, :, 63:64], func=AF.Exp), reads=[K("b")], writes=[KH("dec")])
            P.add("act", lambda e: e.activation(out=d2[:], in_=d2[:], func=AF.Exp), reads=[K("d2")], writes=[K("d2")])
            if full:
                qi, At, sgt = p_qi[h], p_At[h], p_sg[h]
                P.add("act", lambda e: e.activation(out=xx[:], in_=d1[:], func=AF.Exp, scale=-1.0), reads=[K("d1")], writes=[K("x")])
                P.add("act", lambda e: e.activation(out=d1[:], in_=d1[:], func=AF.Exp), reads=[K("d1"), K("x")], writes=[K("d1")])
                P.add("act", lambda e: e.activation(out=b[:], in_=b[:], func=AF.Exp), reads=[K("b"), K("d2"), K("d1"), KH("dec")], writes=[K("b")])
                pq, pqk = proj(h * 128)
                P.add("act", lambda e: e.activation(out=f[:], in_=pq[:], func=AF.Silu), reads=[pqk, K("f"), K("b")], writes=[K("f")])
                pg, pgk = proj(3072 + h * 128)
                P.add("act", lambda e: e.activation(out=sgt[:], in_=pg[:], func=AF.Sigmoid), reads=[pgk], writes=[KH("sg")])

            def stageB():
                P.add("pool", lambda e: e.tensor_tensor(out=ks[:], in0=kk[:], in1=d2[:], op=ALU.mult), reads=[K("kk"), K("d2")], writes=[K("ks")])
                if full:
                    P.add("pool", lambda e: e.tensor_tensor(out=Kt[:], in0=kk[:], in1=xx[:], op=ALU.mult), reads=[K("kk"), K("x")], writes=[K("Kt")])
                    P.add("pool", lambda e: e.tensor_tensor(out=Qt[:], in0=f[:], in1=d1[:], op=ALU.mult), reads=[K("f"), K("d1")], writes=[K("Qt")])
                    P.add("pool", lambda e: e.tensor_tensor(out=qi[:], in0=f[:], in1=b[:], op=ALU.mult), reads=[K("f"), K("b")], writes=[KH("qi")])
                for pr in range(4):
                    P.add("pe", lambda e, pr=pr: e.transpose(psT[:, pr * 128:(pr + 1) * 128], ks[:, pr * 128:(pr + 1) * 128], ident[:]),
                          reads=[K("ks"), "ident"], writes=["D_psT"])
                P.add("act", lambda e: e.activation(out=ksT[:], in_=psT[:], func=AF.Copy), reads=["D_psT"], writes=[KH("ksT")])
                if full:
                    for pr in range(4):
                        P.add("pe", lambda e, pr=pr: e.matmul(psS[:, pr * 128:(pr + 1) * 128], lhsT=Kt[:, pr * 128:(pr + 1) * 128], rhs=Qt[:, pr * 128:(pr + 1) * 128], start=True, stop=True),
                              reads=[K("Kt"), K("Qt")], writes=["D_psS"])
                    P.add("dve", lambda e: e.tensor_tensor(out=At[:].rearrange("p (a b) -> p a b", b=128), in0=psS[:].rearrange("p (a b) -> p a b", b=128),
                                                           in1=m2[:].unsqueeze(1).to_broadcast([128, 4, 128]), op=ALU.mult), reads=["D_psS", "D_m2"], writes=[KH("At")])
            return stageB

        pend = None
        for h in range(8):
            bi = hc % NB
            hc += 1
            sb_ = head_prologue(h, bi)
            if pend is not None:
                pend()
            pend = sb_
        pend()
        for pr in range(4):
            if full:
                for h in range(8):
                    po_ = pob[h % 2][:, 0:128]
                    pok = "D_pob%d" % (h % 2)
                    P.add("pe", lambda e, po_=po_, pr=pr, h=h: e.matmul(po_, lhsT=Vt[:, pr, h * 128:(h + 1) * 128], rhs=p_At[h][:, pr * 128:(pr + 1) * 128], start=True, stop=True),
                          reads=["D_Vt", "D_At%d" % h], writes=[pok])
                    if h % 2 == 0:
                        P.add("act", lambda e, po_=po_, h=h, pr=pr: e.activation(out=p_o[h][:, pr * 128:(pr + 1) * 128], in_=po_, func=AF.Copy), reads=[pok], writes=["D_o%d" % h])
                    else:
                        P.add("dve", lambda e, po_=po_, h=h, pr=pr: e.tensor_copy(out=p_o[h][:, pr * 128:(pr + 1) * 128], in_=po_), reads=[pok], writes=["D_o%d" % h])
            for cc in range(2):
                c = pr * 2 + cc
                for h in range(8):
                    kvb = pkvb[h % 2]
                    kvk = "D_pkvb%d" % (h % 2)
                    kv = kvb[:, 0:128]
                    if full:
                        oi = kvb[:, 128:192]
                        P.add("pe", lambda e, oi=oi, c=c, h=h: e.matmul(oi, lhsT=stb[:, h, :], rhs=p_qi[h][:, c * 64:(c + 1) * 64], start=True, stop=True),
                              reads=["D_stb%d" % h, "D_qi%d" % h], writes=[kvk])
                    P.add("pe", lambda e, kv=kv, cc=cc, pr=pr, h=h: e.matmul(kv, lhsT=p_ksT[h][cc * 64:(cc + 1) * 64, pr * 128:(pr + 1) * 128],
                                                                            rhs=Vt[cc * 64:(cc + 1) * 64, pr, h * 128:(h + 1) * 128], start=True, stop=True),
                          reads=["D_ksT%d" % h, "D_Vt"], writes=[kvk])
                    if full:
                        P.add("dve", lambda e, oi=oi, h=h, c=c: e.tensor_tensor(out=p_o[h][:, c * 64:(c + 1) * 64], in0=oi, in1=p_o[h][:, c * 64:(c + 1) * 64], op=ALU.add),
                              reads=[kvk, "D_o%d" % h], writes=["D_o%d" % h])
                    P.add("dve", lambda e, kv=kv, h=h, c=c: stt(e, stf[:, h, :], stf[:, h, :], decs[:, h, c:c + 1], kv, op0=ALU.mult, op1=ALU.add),
                          reads=[kvk, "D_stf%d" % h, "D_dec%d" % h], writes=["D_stf%d" % h])
                    if full:
                        P.add("act", lambda e, h=h: e.activation(out=stb[:, h, :], in_=stf[:, h, :], func=AF.Copy), reads=["D_stf%d" % h], writes=["D_stb%d" % h])
        if not full:
            continue
        for h in range(8):
            bi = h % NB
            o_sb = p_o[h]
            sqo = t_Qt[bi]
            rr = t_x[bi]
            K = lambda nm: "D_%s%d" % (nm, bi)
            P.add("act", lambda e, sqo=sqo, o_sb=o_sb: e.activation(out=sqo[:], in_=o_sb[:], func=AF.Square), reads=["D_o%d" % h], writes=[K("Qt")])
            gen = pj[pc % 2]; gk = "D_pj%d" % (pc % 2); pc += 1
            P.add("pe", lambda e, sqo=sqo, gen=gen: e.matmul(gen[:, :], lhsT=ones128[:], rhs=sqo[:], start=True, stop=True), reads=[K("Qt"), "ones_d128"], writes=[gk])
            P.add("act", lambda e, rr=rr, gen=gen: e.activation(out=rr[:], in_=gen[:], func=AF.Sqrt, bias=EPS, scale=1.0), reads=[gk], writes=[K("x")])
            P.add("dve", lambda e, rr=rr: e.reciprocal(out=rr[:], in_=rr[:]), reads=[K("x")], writes=[K("x")])
            P.add("dve", lambda e, o_sb=o_sb, rr=rr: stt(e, o_sb[:], o_sb[:], tabs[:, T_HOG:T_HOG + 1], rr[:]), reads=["D_o%d" % h, K("x"), "tabs"], writes=["D_o%d" % h])
            P.add("pool", lambda e, o_sb=o_sb, h=h: e.tensor_tensor(out=gated[:, h, :], in0=o_sb[:], in1=p_sg[h][:], op=ALU.mult), reads=["D_o%d" % h, "D_sg%d" % h, "D_sq"], writes=["D_gated%d" % h])
        gkeys = ["D_gated%d" % h for h in range(8)]
        for oc in range(8):
            pp = pj[pc % 2]; pk = "D_pj%d" % (pc % 2); pc += 1
            for c in range(8):
                P.add("pe", lambda e, c=c, oc=oc, pp=pp: e.matmul(pp[:, :], lhsT=wo[:, c, oc * 128:(oc + 1) * 128], rhs=gated[:, c, :], start=(c == 0), stop=(c == 7)),
                      reads=["D_wo"] + gkeys, writes=[pk])
            P.add("dve", lambda e, oc=oc, pp=pp: e.tensor_tensor(out=ht[:, oc, :], in0=pp[:], in1=ht[:, oc, :], op=ALU.add), reads=[pk, "D_ht%d" % oc], writes=["D_ht%d" % oc])
        if write_h:
            P.dma(ov[:, :, j * N:(j + 1) * N], ht[:], reads=hkeys + gkeys)
    P.dma(d["s_out"].rearrange("h k v -> k h v"), stf[:], reads=skeys)


def phase_E(P, C, d, n_exp=8):
    tabs = C.tabs
    N = 512
    TG = 2048
    onesm = C.ones_scaled(1.0 / 1024, "d1024")
    identf = P.sb("E_identf", [128, 128], F32)
    P.add("dve", lambda e: e.tensor_copy(out=identf[:], in_=C.ident[:]), reads=["ident"], writes=["E_identf"])
    sel = P.sb("E_sel", [8, 8, 128], F32)
    P.add("pool", lambda e: e.memset(sel[:], 1.0), writes=["E_sel"])
    P.add("pool", lambda e: e.affine_select(out=sel[:], in_=sel[:], pattern=[[1, 8], [0, 128]], compare_op=ALU.is_equal, fill=0.0, base=0, channel_multiplier=-1),
          reads=["E_sel"], writes=["E_sel"])
    wr = P.sb("E_wr", [128, 8, 8], F32)
    P.dma(wr[:], d["moe_w_router"].rearrange("(c p) e -> p c e", p=128), writes=["E_wr"])
    acc = P.sb("E_acc", [128, 8, TG], F32)
    xn = P.sb("E_xn", [128, 8, TG], BF16)
    xf = P.sb("E_xf", [128, 8, N], F32)
    sq = P.sb("E_sq", [128, 8, N], BF16)
    sd = P.sb("E_sd", [128, N], F32)
    rstd = P.sb("E_rstd", [128, N], F32)
    gT = P.sb("E_gT", [8, TG], F32)
    gbt = P.sb("E_gbt", [128, TG], BF16)
    lg = P.sb("E_lg", [128, 8], F32)
    eq1 = P.sb("E_eq1", [128, 8], F32)
    eq2 = P.sb("E_eq2", [128, 8], F32)
    l2 = P.sb("E_l2", [128, 8], F32)
    m1 = P.sb("E_m1", [128, 1], F32)
    m2 = P.sb("E_m2", [128, 1], F32)
    g1 = P.sb("E_g1", [128, 1], F32)
    g2 = P.sb("E_g2", [128, 1], F32)
    gts = P.sb("E_gts", [128, 8], F32)
    wgb = [P.sb("E_wg%d" % i, [128, 8, 512], BF16) for i in range(2)]
    wub = [P.sb("E_wu%d" % i, [128, 8, 512], BF16) for i in range(2)]
    wdb = [P.sb("E_wd%d" % i, [128, 4, 1024], BF16) for i in range(2)]
    ab = [P.sb("E_a%d" % i, [128, 4, N], BF16) for i in range(2)]
    sgb = [P.sb("E_sg%d" % i, [128, N], BF16) for i in range(2)]
    t1b = [P.sb("E_t1%d" % i, [128, N], BF16) for i in range(2)]
    pgu = [P.ps("E_pgu%d" % i, [128, 512]) for i in range(4)]
    pdn = [P.ps("E_pdn%d" % i, [128, 512]) for i in range(2)]
    gen = P.ps("E_gen", [128, 512])
    pss = P.ps("E_pss", [128, 512])
    hv = d["h3T"].rearrange("(c p) t -> p c t", p=128)
    ov = d["outT"].rearrange("(c p) t -> p c t", p=128)
    wc = 0
    uc = 0
    dc = 0
    for tg in range(4096 // TG):
        for t in range(TG // N):
            cols = slice(t * N, (t + 1) * N)
            akeys = ["E_acc%d_%d" % (c, t) for c in range(8)]
            P.dma(acc[:, :, cols], hv[:, :, tg * TG + t * N:tg * TG + (t + 1) * N], writes=akeys)
            P.add("act", lambda e, cols=cols: e.activation(out=sq[:], in_=acc[:, :, cols], func=AF.Square), reads=akeys, writes=["E_sq"])
            for c in range(8):
                P.add("pe", lambda e, c=c: e.matmul(pss[:, :], lhsT=onesm[:], rhs=sq[:, c, :], start=(c == 0), stop=(c == 7)), reads=["E_sq", "ones_d1024"], writes=["E_pss"])
            P.add("act", lambda e: e.activation(out=sd[:], in_=pss[:], func=AF.Sqrt, bias=EPS, scale=1.0), reads=["E_pss"], writes=["E_sd"])
            P.add("dve", lambda e: e.reciprocal(out=rstd[:], in_=sd[:]), reads=["E_sd"], writes=["E_rstd"])
            for c in range(8):
                P.add("dve", lambda e, c=c, cols=cols: stt(e, xf[:, c, :], acc[:, c, cols], tabs[:, T_MOE_NORM + c:T_MOE_NORM + c + 1], rstd[:]),
                      reads=["E_acc%d_%d" % (c, t), "E_rstd", "tabs"], writes=["E_xf%d" % c])
                P.add("act", lambda e, c=c, cols=cols: e.activation(out=xn[:, c, cols], in_=xf[:, c, :], func=AF.Copy), reads=["E_xf%d" % c], writes=["E_xn%d_%d" % (c, t)])
            fkeys = ["E_xf%d" % c for c in range(8)]
            for tb in range(4):
                for c in range(8):
                    P.add("pe", lambda e, c=c, tb=tb: e.matmul(gen[:, 0:8], lhsT=xf[:, c, tb * 128:(tb + 1) * 128], rhs=wr[:, c, :], start=(c == 0), stop=(c == 7)),
                          reads=fkeys + ["E_wr"], writes=["E_gen"])
                P.add("dve", lambda e: e.tensor_copy(out=lg[:], in_=gen[:, 0:8]), reads=["E_gen"], writes=["E_lg"])
                P.add("dve", lambda e: e.reduce_max(out=m1[:], in_=lg[:], axis=AX.X), reads=["E_lg"], writes=["E_m1"])
                P.add("dve", lambda e: e.tensor_tensor(out=eq1[:], in0=lg[:], in1=m1[:, 0:1].to_broadcast([128, 8]), op=ALU.is_equal), reads=["E_lg", "E_m1"], writes=["E_eq1"])
                P.add("dve", lambda e: stt(e, l2[:], eq1[:], -1e30, lg[:], op0=ALU.mult, op1=ALU.add), reads=["E_eq1", "E_lg"], writes=["E_l2"])
                P.add("dve", lambda e: e.reduce_max(out=m2[:], in_=l2[:], axis=AX.X), reads=["E_l2"], writes=["E_m2"])
                P.add("dve", lambda e: e.tensor_tensor(out=eq2[:], in0=l2[:], in1=m2[:, 0:1].to_broadcast([128, 8]), op=ALU.is_equal), reads=["E_l2", "E_m2"], writes=["E_eq2"])
                P.add("dve", lambda e: e.tensor_tensor(out=g1[:], in0=m2[:], in1=m1[:], op=ALU.subtract), reads=["E_m1", "E_m2"], writes=["E_g1"])
                P.add("act", lambda e: e.activation(out=g1[:], in_=g1[:], func=AF.Exp), reads=["E_g1"], writes=["E_g1"])
                P.add("dve", lambda e: e.tensor_scalar(out=g1[:], in0=g1[:], scalar1=1.0, scalar2=0.0, op0=ALU.add, op1=ALU.add), reads=["E_g1"], writes=["E_g1"])
                P.add("dve", lambda e: e.reciprocal(out=g1[:], in_=g1[:]), reads=["E_g1"], writes=["E_g1"])
                P.add("dve", lambda e: e.tensor_scalar(out=g2[:], in0=g1[:], scalar1=-1.0, scalar2=1.0, op0=ALU.mult, op1=ALU.add), reads=["E_g1"], writes=["E_g2"])
                P.add("dve", lambda e: e.tensor_scalar(out=gts[:], in0=eq1[:], scalar1=g1[:, 0:1], scalar2=0.0, op0=ALU.mult, op1=ALU.add), reads=["E_eq1", "E_g1"], writes=["E_gts"])
                P.add("dve", lambda e: stt(e, gts[:], eq2[:], g2[:, 0:1], gts[:], op0=ALU.mult, op1=ALU.add), reads=["E_eq2", "E_g2", "E_gts"], writes=["E_gts"])
                P.add("pe", lambda e: e.transpose(gen[0:8, 128:256], gts[:, :], identf[:]), reads=["E_gts", "E_identf"], writes=["E_gen"])
                c0 = t * N + tb * 128
                P.add("act", lambda e, c0=c0: e.activation(out=gT[:, c0:c0 + 128], in_=gen[0:8, 128:256], func=AF.Copy), reads=["E_gen"], writes=["E_gT"])
        xkeys_t = [["E_xn%d_%d" % (c, t) for c in range(8)] for t in range(TG // N)]
        units = [(ex, fg) for ex in range(n_exp) for fg in range(7)]

        def w_chunks(u):
            ex, fg = units[u]
            wi = u % 2
            wg, wu, wd = wgb[wi], wub[wi], wdb[wi]
            lst = []
            for c in range(8):
                lst.append((wg[:, c, :], d["moe_w_gate"][ex, c * 128:(c + 1) * 128, fg * 512:(fg + 1) * 512], "E_wg%d" % wi))
            for c in range(8):
                lst.append((wu[:, c, :], d["moe_w_up"][ex, c * 128:(c + 1) * 128, fg * 512:(fg + 1) * 512], "E_wu%d" % wi))
            for c in range(4):
                for hf in range(2):
                    lst.append((wd[:, c, hf * 512:(hf + 1) * 512], d["moe_w_down"][ex, fg * 512 + c * 128:fg * 512 + (c + 1) * 128, hf * 512:(hf + 1) * 512], "E_wd%d" % wi))
            return lst

        slot_ctr = [0]

        def load_group(u, gi, do_dma=True, do_cast=True, _st={}):
            lst = w_chunks(u)[7 * gi:7 * gi + 7]
            if do_dma:
                sl = []
                for (dst, src, key) in lst:
                    si_ = slot_ctr[0] % 8
                    slot_ctr[0] += 1
                    P.dma(xf[:, si_, :], src, writes=["E_xf%d" % si_])
                    sl.append(si_)
                _st[(u, gi)] = sl
            if do_cast:
                for (dst, src, key), si_ in zip(lst, _st[(u, gi)]):
                    P.add("act", lambda e, dst=dst, si_=si_: e.activation(out=dst, in_=xf[:, si_, :], func=AF.Copy), reads=["E_xf%d" % si_], writes=[key])

        def load_w(u):
            for gi in range(4):
                load_group(u, gi)

        load_w(0)
        pend_down = [None]
        for u, (ex, fg) in enumerate(units):
            if fg == 0:
                for t in range(TG // N):
                    P.add("pe", lambda e, ex=ex, t=t: e.matmul(gen[:, :], lhsT=sel[:, ex, :], rhs=gT[:, t * N:(t + 1) * N], start=True, stop=True), reads=["E_sel", "E_gT"], writes=["E_gen"])
                    P.add("act", lambda e, t=t: e.activation(out=gbt[:, t * N:(t + 1) * N], in_=gen[:], func=AF.Copy), reads=["E_gen"], writes=["E_gbt"])
            if True:
                wi = u % 2
                wg, wu, wd = wgb[wi], wub[wi], wdb[wi]
                for t in range(TG // N):
                    cols = slice(t * N, (t + 1) * N)
                    ai = uc % 2
                    a = ab[ai]
                    if u + 1 < len(units):
                        load_group(u + 1, t, do_dma=True, do_cast=False)
                    for fc in range(4):
                        pg = pgu[(uc * 8 + fc * 2) % 4]; pgk = "E_pgu%d" % ((uc * 8 + fc * 2) % 4)
                        pu = pgu[(uc * 8 + fc * 2 + 1) % 4]; puk = "E_pgu%d" % ((uc * 8 + fc * 2 + 1) % 4)
                        for c in range(8):
                            P.add("pe", lambda e, pg=pg, c=c, fc=fc, wg=wg, cols=cols: e.matmul(pg[:, :], lhsT=wg[:, c, fc * 128:(fc + 1) * 128], rhs=xn[:, c, cols], start=(c == 0), stop=(c == 7)),
                                  reads=["E_wg%d" % wi] + xkeys_t[t], writes=[pgk])
                        for c in range(8):
                            P.add("pe", lambda e, pu=pu, c=c, fc=fc, wu=wu, cols=cols: e.matmul(pu[:, :], lhsT=wu[:, c, fc * 128:(fc + 1) * 128], rhs=xn[:, c, cols], start=(c == 0), stop=(c == 7)),
                                  reads=["E_wu%d" % wi] + xkeys_t[t], writes=[puk])
                        si = fc % 2
                        P.add("act", lambda e, pg=pg, si=si: e.activation(out=sgb[si][:], in_=pg[:], func=AF.Silu), reads=[pgk], writes=["E_sg%d" % si])
                        P.add("pool", lambda e, si=si, cols=cols: e.tensor_tensor(out=t1b[si][:], in0=sgb[si][:], in1=gbt[:, cols], op=ALU.mult), reads=["E_sg%d" % si, "E_gbt"], writes=["E_t1%d" % si])
                        P.add("dve", lambda e, pu=pu, si=si, a=a, fc=fc: e.tensor_tensor(out=a[:, fc, :], in0=pu[:], in1=t1b[si][:], op=ALU.mult), reads=[puk, "E_t1%d" % si], writes=["E_a%d_%d" % (ai, fc)])
                    uc += 1

                    def down(a=a, ai=ai, wd=wd, wi=wi, cols=cols, t=t):
                        nonlocal dc
                        for oc in range(8):
                            pd = pdn[dc % 2]; pdk = "E_pdn%d" % (dc % 2); dc += 1
                            for fc in range(4):
                                P.add("pe", lambda e, pd=pd, fc=fc, oc=oc: e.matmul(pd[:, :], lhsT=wd[:, fc, oc * 128:(oc + 1) * 128], rhs=a[:, fc, :], start=(fc == 0), stop=(fc == 3)),
                                      reads=["E_wd%d" % wi] + ["E_a%d_%d" % (ai, f_) for f_ in range(4)], writes=[pdk])
                            P.add("dve", lambda e, pd=pd, oc=oc: e.tensor_tensor(out=acc[:, oc, cols], in0=pd[:], in1=acc[:, oc, cols], op=ALU.add),
                                  reads=[pdk, "E_acc%d_%d" % (oc, t)], writes=["E_acc%d_%d" % (oc, t)])
                    if pend_down[0] is not None:
                        pend_down[0]()
                    pend_down[0] = down
                    if u + 1 < len(units):
                        load_group(u + 1, t, do_dma=False, do_cast=True)
        pend_down[0]()
        pend_down[0] = None
        P.dma(ov[:, :, tg * TG:(tg + 1) * TG], acc[:], reads=["E_acc%d_%d" % (c, t) for c in range(8) for t in range(4)])


SCR = [("qd", [768, 4096]), ("kd", [768, 6144]), ("qf1", [256, 4096]), ("qf2", [256, 4096]), ("kf", [256, 8192]),
       ("vd", [12, 128, 48, 65]), ("vf", [4, 128, 64, 65])]
W_IN = [("attn_w_in", [1024, 3072]), ("attn_w_out", [1024, 1024]), ("ffn_w_gate", [1024, 2816]), ("ffn_w_up", [1024, 2816]),
        ("ffn_w_down", [2816, 1024]), ("hgrn_w_in", [1024, 4096]), ("hgrn_w_out", [1024, 1024]), ("moe_w_router", [1024, 8]),
        ("moe_w_gate", [8, 1024, 3584]), ("moe_w_up", [8, 1024, 3584]), ("moe_w_down", [8, 3584, 1024])]


def build_fused(n_cores=8, phases="ABCDE", n_exp=8):
    nc = bass.Bass("TRN2", target_bir_lowering=False)
    d = {}
    tabs = nc.dram_tensor("tabs", [128, T_NT], F32, kind="ExternalInput").ap()
    d["xT"] = nc.dram_tensor("xT", [1024, 8192], F32, kind="ExternalInput").ap()
    for n, s in W_IN:
        d[n] = nc.dram_tensor(n, s, F32, kind="ExternalInput").ap()
    d["outT"] = nc.dram_tensor("outT", [1024, 4096], F32, kind="ExternalOutput").ap()
    for n, s in SCR:
        d[n] = nc.dram_tensor(n, s, BF16).ap()
    d["mixT"] = nc.dram_tensor("mixT", [1024, 4096], BF16).ap()
    d["h2T"] = nc.dram_tensor("h2T", [1024, 4096], F32).ap()
    d["h3T"] = nc.dram_tensor("h3T", [1024, 4096], F32).ap()
    s_a = nc.dram_tensor("s_a", [1024, 128], F32).ap()
    s_g = nc.dram_tensor("s_g", [2048, 128], F32).ap()
    s_dummy = nc.dram_tensor("s_dummy", [1024, 128], F32).ap()

    def run_phase(tag, fn):
        with nc.cleanup_on_exit():
            P = Prog(nc, tag=tag, fused=True)
            C = Ctx(P, tabs)
            fn(P, C)
            P.emit()
            nc.all_engine_barrier()

    if "A" in phases:
        run_phase("A_", lambda P, C: phase_A(P, C, d))
    if "B" in phases:
        run_phase("B_", lambda P, C: phase_B(P, C, d))
    if "C" in phases:
        run_phase("C_", lambda P, C: phase_C(P, C, d))
    if "D" in phases:
        d1 = dict(d); d1["s_out"] = s_a.rearrange("(h k) v -> h k v", h=8)
        run_phase("D1_", lambda P, C: phase_D(P, C, d1, zero_init=True, write_h=False, state_only=True))
        groups = [[2 * i, 2 * i + 1] for i in range(n_cores // 2)]
        with nc.cleanup_on_exit():
            cc_sem = nc.alloc_semaphore("cc_sem")
            with nc.Block() as block:
                @block.gpsimd
                def _(g):
                    g.collective_compute("AllGather", ALU.bypass, replica_groups=groups, ins=[s_a], outs=[s_g]).then_inc(cc_sem)
                    g.wait_ge(cc_sem, 1)
            nc.all_engine_barrier()
        d2 = dict(d); d2["s_in"] = s_g[0:1024, :].rearrange("(h k) v -> h k v", h=8); d2["s_out"] = s_dummy.rearrange("(h k) v -> h k v", h=8)
        run_phase("D2_", lambda P, C: phase_D(P, C, d2, scale_pv=True))
    if "E" in phases:
        run_phase("E_", lambda P, C: phase_E(P, C, d, n_exp=n_exp))
    return nc


def make_tabs(inp, pv):
    t = np.zeros((128, T_NT), np.float32)
    p = np.arange(128)
    def pc(v):
        return np.asarray(v, np.float32).reshape(8, 128).T
    t[:, T_ATTN_NORM:T_ATTN_NORM + 8] = pc(inp["attn_norm"][0])
    t[:, T_FFN_NORM:T_FFN_NORM + 8] = pc(inp["ffn_norm"][0])
    t[:, T_HGRN_NORM:T_HGRN_NORM + 8] = pc(inp["hgrn_norm"][0])
    t[:, T_MOE_NORM:T_MOE_NORM + 8] = pc(inp["moe_norm"][0])
    t[:, T_DQG] = inp["dil_q_gain"][0][p % 64]
    t[:, T_DKG] = inp["dil_k_gain"][0][p % 64]
    t[:, T_FQG] = inp["diff_q_gain"][0][p % 32]
    t[:, T_FKG] = inp["diff_k_gain"][0][p % 32]
    t[:, T_DOG] = inp["diff_out_gain"][0][p % 64]
    t[:, T_HOG] = inp["hgrn_out_gain"][0]
    t[:, T_PV] = pv
    t[:, T_M1] = (p % 64 < 32)
    t[:, T_M2] = (p % 64 >= 32)
    t[:, T_LB0:T_LB0 + 8] = pc(inp["hgrn_lb_logits"][0])
    t[:, T_LB1:T_LB1 + 8] = pc(inp["hgrn_lb_logits"][1])
    for i, k in enumerate(["diff_lambda_q1", "diff_lambda_k1", "diff_lambda_q2", "diff_lambda_k2"]):
        t[:32, T_LQ1 + i] = inp[k][0]
    return t

def core_xT(x, core):
    b, hf = core // 2, core % 2
    xb = x[b]
    out = np.zeros((1024, 8192), np.float32)
    if hf == 1:
        out[:, :4096] = xb[:4096].T
        out[:, 4096:] = xb[4096:].T
    else:
        out[:, 4096:] = xb[:4096].T
    return out


_NC = {}


def kernel(**inp):
    inp = {k: np.asarray(v) for k, v in inp.items()}
    NCORE = 8
    cores = list(range(NCORE))
    if "nc" not in _NC:
        _NC["nc"] = build_fused(NCORE)
    maps = []
    for c in cores:
        m = {"tabs": make_tabs(inp, c % 2), "xT": core_xT(inp["x"], c)}
        for n, s in W_IN:
            m[n] = np.ascontiguousarray(inp[n][0])
        maps.append(m)
    res = run_bass_kernel_spmd(_NC["nc"], maps, core_ids=cores).results
    out = np.empty((4, 8192, 1024), np.float32)
    for c in cores:
        out[c // 2, (c % 2) * 4096:(c % 2 + 1) * 4096, :] = np.asarray(res[c]["outT"]).T
    return out
```
